# Optimizing a Trainium2 kernel written in Bass

```python
import jax, jax.numpy as jnp
from jax import lax
import numpy as np

D_MODEL = 1024
BATCH = 4
SEQ = 4096
DEPTH = 2

N_BRANCH = 4
BRANCH_WIDTH = 256
SGU_HEADS = 4
SGU_HEAD_DIM = BRANCH_WIDTH // SGU_HEADS
SGU_CHUNK = 128
CONV_WIDTH = 3
POOL_WINDOWS = (2, 4, 8, 16)
POOL_GROUPS = len(POOL_WINDOWS)
POOL_GROUP_DIM = BRANCH_WIDTH // POOL_GROUPS
GLA_HEADS = 4
GLA_DK = 32
GLA_DV = BRANCH_WIDTH // GLA_HEADS
GLA_RANK = 16
GLA_TAU = 16.0
GLA_CHUNK = 64
D_FF = 2816
N_EXPERTS = 8
TOP_K = 2
EPS = 1e-6

A_COLS = 2 * BRANCH_WIDTH
B_COLS = 3 * BRANCH_WIDTH
C_COLS = BRANCH_WIDTH
D_COLS = 2 * GLA_HEADS * GLA_DK + 2 * GLA_HEADS * GLA_DV + GLA_RANK
G_COLS = N_BRANCH * D_MODEL
IN_COLS = A_COLS + B_COLS + C_COLS + D_COLS + G_COLS
SPLITS = (A_COLS, A_COLS + B_COLS, A_COLS + B_COLS + C_COLS, A_COLS + B_COLS + C_COLS + D_COLS)
N_DENSE = (DEPTH + 1) // 2
N_MOE = DEPTH // 2

kernel_name = "hybrid_gated_multimixer_moe_trunk"


def rmsnorm(x, g):
    x32 = x.astype(jnp.float32)
    y = x32 * lax.rsqrt(jnp.mean(x32 * x32, axis=-1, keepdims=True) + EPS)
    return (y * g.astype(jnp.float32)).astype(x.dtype)


def sgu_mixer(za, sgu_w, sgu_b, sgu_norm):
    z = jax.nn.gelu(za)
    u, v = jnp.split(z, 2, axis=-1)
    b, s, _ = v.shape
    nc = s // SGU_CHUNK
    v = rmsnorm(v.reshape(b, s, SGU_HEADS, SGU_HEAD_DIM), sgu_norm.reshape(SGU_HEADS, SGU_HEAD_DIM))
    v = v.reshape(b, nc, SGU_CHUNK, SGU_HEADS, SGU_HEAD_DIM)
    causal = jnp.tril(jnp.ones((SGU_CHUNK, SGU_CHUNK), dtype=bool))
    w = jnp.where(causal[None], sgu_w, 0)
    mixed = jnp.einsum('hts,bnshd->bnthd', w, v) + sgu_b.T[None, None, :, :, None]
    return u * mixed.reshape(b, s, BRANCH_WIDTH)


def shortconv_mixer(zb, conv_w, conv_b):
    xin, gate_b, gate_c = jnp.split(zb, 3, axis=-1)
    y = gate_c * xin
    s = y.shape[1]
    yp = jnp.pad(y, ((0, 0), (CONV_WIDTH - 1, 0), (0, 0)))
    conv = conv_b + yp[:, 0:s] * conv_w[0]
    for i in range(1, CONV_WIDTH):
        conv = conv + yp[:, i:i + s] * conv_w[i]
    return gate_b * conv


def pool_mixer(zc, pool_w, pool_scale):
    b, s, _ = zc.shape
    z32 = zc.astype(jnp.float32)
    incl = jnp.cumsum(z32, axis=1)
    t = jnp.arange(s)
    outs = []
    for g, w in enumerate(POOL_WINDOWS):
        sl = slice(g * POOL_GROUP_DIM, (g + 1) * POOL_GROUP_DIM)
        c = incl[..., sl]
        lag = jnp.pad(c[:, :s - w], ((0, 0), (w, 0), (0, 0)))
        count = jnp.minimum(t + 1, w).astype(jnp.float32)[None, :, None]
        outs.append((c - lag) / count - z32[..., sl])
    pooled = jnp.stack(outs, axis=2).astype(zc.dtype)
    mixed = jnp.einsum('bsgc,gcd->bsgd', pooled, pool_w)
    return mixed.reshape(b, s, BRANCH_WIDTH) * pool_scale


def gla_mixer(zd, gla_wg2, gla_bg, gla_norm):
    b, s, _ = zd.shape
    hk = GLA_HEADS * GLA_DK
    hv = GLA_HEADS * GLA_DV
    q, k, v, r, glr = jnp.split(zd, [hk, 2 * hk, 2 * hk + hv, 2 * hk + 2 * hv], axis=-1)
    log_g = jax.nn.log_sigmoid((glr @ gla_wg2 + gla_bg).astype(jnp.float32)) / GLA_TAU
    nc = s // GLA_CHUNK

    def chunked(a, d):
        return a.reshape(b, nc, GLA_CHUNK, GLA_HEADS, d).astype(jnp.float32)

    q = chunked(q, GLA_DK) * (GLA_DK ** -0.5)
    k = chunked(k, GLA_DK)
    v = chunked(v, GLA_DV)
    cum = jnp.cumsum(chunked(log_g, GLA_DK), axis=2)
    last = cum[:, :, -1]
    q_dec = q * jnp.exp(cum)
    k_inv = k * jnp.exp(-cum)
    k_end = k * jnp.exp(last[:, :, None] - cum)
    causal = jnp.tril(jnp.ones((GLA_CHUNK, GLA_CHUNK), dtype=bool))
    scores = jnp.where(causal, jnp.einsum('bnthk,bnshk->bnhts', q_dec, k_inv), 0.0)
    o_intra = jnp.einsum('bnhts,bnshv->bnthv', scores, v)
    kv = jnp.einsum('bnshk,bnshv->bnhkv', k_end, v)
    decay = jnp.exp(last)

    def step(state, inp):
        kv_n, dec_n = inp
        return dec_n[..., None] * state + kv_n, state

    init = jnp.zeros((b, GLA_HEADS, GLA_DK, GLA_DV), jnp.float32)
    _, states = lax.scan(step, init, (jnp.moveaxis(kv, 1, 0), jnp.moveaxis(decay, 1, 0)))
    states = jnp.moveaxis(states, 0, 1)
    o_inter = jnp.einsum('bnthk,bnhkv->bnthv', q_dec, states)
    o = rmsnorm((o_intra + o_inter).reshape(b, s, GLA_HEADS, GLA_DV), gla_norm)
    return jax.nn.silu(r) * o.reshape(b, s, hv).astype(zd.dtype)


def mixer_block(h, w_in, b_gate, sgu_w, sgu_b, sgu_norm, conv_w, conv_b, pool_w, pool_scale,
                gla_wg2, gla_bg, gla_norm, branch_proj, w_out):
    b, s, _ = h.shape
    z = h @ w_in
    za, zb, zc, zd, zg = jnp.split(z, SPLITS, axis=-1)
    ya = sgu_mixer(za, sgu_w, sgu_b, sgu_norm)
    yb = shortconv_mixer(zb, conv_w, conv_b)
    yc = pool_mixer(zc, pool_w, pool_scale)
    yd = gla_mixer(zd, gla_wg2, gla_bg, gla_norm)
    branches = jnp.stack([ya, yb, yc, yd], axis=2)
    proj = jnp.einsum('bsiw,iwd->bsid', branches, branch_proj)
    gates = jax.nn.sigmoid(zg.reshape(b, s, N_BRANCH, D_MODEL) + b_gate)
    merged = jnp.sum(gates * proj, axis=2)
    return merged @ w_out


def swiglu(t, w1, w3, w2):
    return (jax.nn.silu(t @ w1) * (t @ w3)) @ w2


def moe_swiglu(h, router, w1, w3, w2):
    b, s, d = h.shape
    t = h.reshape(b * s, d)
    logits = (t @ router).astype(jnp.float32)
    top_vals, top_idx = lax.top_k(logits, TOP_K)
    weights = jax.nn.softmax(top_vals, axis=-1)
    combine = jnp.sum(jax.nn.one_hot(top_idx, N_EXPERTS, dtype=jnp.float32) * weights[..., None], axis=1)
    combine = combine.astype(t.dtype)
    out = combine[:, 0:1] * swiglu(t, w1[0], w3[0], w2[0])
    for e in range(1, N_EXPERTS):
        out = out + combine[:, e:e + 1] * swiglu(t, w1[e], w3[e], w2[e])
    return out.reshape(b, s, d)


def setup_inputs(seed: int = 0) -> dict:
    key = jax.random.key(seed)
    ks = jax.random.split(key, 25)
    L = DEPTH
    f32 = jnp.float32

    def nrm(k, shape, scale):
        return jax.random.normal(k, shape, f32) * scale

    def gain(k, shape):
        return 1.0 + 0.01 * jax.random.normal(k, shape, f32)

    return {
        "x": nrm(ks[0], (BATCH, SEQ, D_MODEL), 1.0),
        "norm_mix": gain(ks[1], (L, D_MODEL)),
        "w_in": nrm(ks[2], (L, D_MODEL, IN_COLS), D_MODEL ** -0.5),
        "b_gate": nrm(ks[3], (L, N_BRANCH, D_MODEL), 0.01),
        "sgu_w": nrm(ks[4], (L, SGU_HEADS, SGU_CHUNK, SGU_CHUNK), SGU_CHUNK ** -0.5),
        "sgu_b": gain(ks[5], (L, SGU_HEADS, SGU_CHUNK)),
        "sgu_norm": gain(ks[6], (L, BRANCH_WIDTH)),
        "conv_w": nrm(ks[7], (L, CONV_WIDTH, BRANCH_WIDTH), CONV_WIDTH ** -0.5),
        "conv_b": nrm(ks[8], (L, BRANCH_WIDTH), 0.01),
        "pool_w": nrm(ks[9], (L, POOL_GROUPS, POOL_GROUP_DIM, POOL_GROUP_DIM), POOL_GROUP_DIM ** -0.5),
        "pool_scale": gain(ks[10], (L, BRANCH_WIDTH)),
        "gla_wg2": nrm(ks[11], (L, GLA_RANK, GLA_HEADS * GLA_DK), GLA_RANK ** -0.5),
        "gla_bg": nrm(ks[12], (L, GLA_HEADS * GLA_DK), 0.01),
        "gla_norm": gain(ks[13], (L, GLA_DV)),
        "branch_proj": nrm(ks[14], (L, N_BRANCH, BRANCH_WIDTH, D_MODEL), BRANCH_WIDTH ** -0.5),
        "w_out": nrm(ks[15], (L, D_MODEL, D_MODEL), D_MODEL ** -0.5),
        "norm_ffn": gain(ks[16], (L, D_MODEL)),
        "ffn_w1": nrm(ks[17], (N_DENSE, D_MODEL, D_FF), D_MODEL ** -0.5),
        "ffn_w3": nrm(ks[18], (N_DENSE, D_MODEL, D_FF), D_MODEL ** -0.5),
        "ffn_w2": nrm(ks[19], (N_DENSE, D_FF, D_MODEL), D_FF ** -0.5),
        "moe_router": nrm(ks[20], (N_MOE, D_MODEL, N_EXPERTS), D_MODEL ** -0.5),
        "moe_w1": nrm(ks[21], (N_MOE, N_EXPERTS, D_MODEL, D_FF), D_MODEL ** -0.5),
        "moe_w3": nrm(ks[22], (N_MOE, N_EXPERTS, D_MODEL, D_FF), D_MODEL ** -0.5),
        "moe_w2": nrm(ks[23], (N_MOE, N_EXPERTS, D_FF, D_MODEL), D_FF ** -0.5),
        "final_norm": gain(ks[24], (D_MODEL,)),
    }


def reference(x, norm_mix, w_in, b_gate, sgu_w, sgu_b, sgu_norm, conv_w, conv_b, pool_w, pool_scale,
              gla_wg2, gla_bg, gla_norm, branch_proj, w_out, norm_ffn, ffn_w1, ffn_w3, ffn_w2,
              moe_router, moe_w1, moe_w3, moe_w2, final_norm):
    for l in range(DEPTH):
        h = rmsnorm(x, norm_mix[l])
        x = x + mixer_block(h, w_in[l], b_gate[l], sgu_w[l], sgu_b[l], sgu_norm[l], conv_w[l], conv_b[l],
                            pool_w[l], pool_scale[l], gla_wg2[l], gla_bg[l], gla_norm[l],
                            branch_proj[l], w_out[l])
        h = rmsnorm(x, norm_ffn[l])
        if l % 2 == 0:
            i = l // 2
            x = x + swiglu(h, ffn_w1[i], ffn_w3[i], ffn_w2[i])
        else:
            i = l // 2
            x = x + moe_swiglu(h, moe_router[i], moe_w1[i], moe_w3[i], moe_w2[i])
    return rmsnorm(x, final_norm)
```

```python
import numpy as np
from contextlib import ExitStack
import concourse.bass as bass
import concourse.mybir as mybir
from concourse.bass_utils import run_bass_kernel_spmd

F32 = mybir.dt.float32
BF16 = mybir.dt.bfloat16
AF = mybir.ActivationFunctionType
ALU = mybir.AluOpType
AX = mybir.AxisListType

NCORES = 8
D = 1024
T = 2048
NT = 16
NB = 4
DFF = 2816
NE = 8
EPS = 1e-6
L = 2
NSLOT = 3
SLOT = 4096

OB_SGUB = 0
OB_SGUN = 256
OB_GLAN = 512
OB_WST = 768
OB_POOLW = 1280
OB_BG = 1536
OB_CONVW = 1664
OB_CONVB = 1670
OB_PSC = 1672
OB_INVW = 1674
NSB = 1676
OP_GMIX = 0
OP_GFFN = 16
OP_BGATE = 32
OP_FIN = 96
OP_ROUTER = 104
NSP = 168
OC_IDENT = 0
OC_TRISGU = 128
OC_CAUS2 = 256
OC_BMASK = 384
OC_TRI2 = 640
OC_REV2 = 768
OC_CH2 = 896
OC_ONES = 898
OC_HM = 1026
NC = 1030
NPC = 72
XW = 100


class Tok:
    __slots__ = ("src", "idx")

    def __init__(self, src, idx):
        self.src = src
        self.idx = idx


class _Rec:
    def __init__(self):
        self.call = None

    def __getattr__(self, name):
        def f(*a, **k):
            assert self.call is None
            self.call = (name, a, k)
            return self
        return f


def _eager(fn):
    r = _Rec()
    fn(r)
    name, a, k = r.call
    return lambda eng: getattr(eng, name)(*a, **k)


class Builder:
    ENGS = ("pe", "act", "dve", "pool", "sp")

    def __init__(self):
        self.q = {e: [] for e in self.ENGS}
        self.nops = {}
        self.ops = {}
        self.waited = {e: {} for e in self.ENGS}
        self.lw = {}
        self.rd = {}
        self.dma_sems = set()
        self.cc_sems = set()

    def _wait(self, e, tok):
        if tok.src == e and e == "pe":
            return
        w = self.waited[e]
        if w.get(tok.src, 0) >= tok.idx:
            return
        w[tok.src] = tok.idx
        self.q[e].append(["wait", tok.src, tok.idx])
        self.ops[tok.src][tok.idx - 1][2] = True

    def _deps(self, e, reads, writes, extra):
        lw, rd = self.lw, self.rd
        for k in reads:
            t = lw.get(k)
            if t is not None:
                self._wait(e, t)
        for k in writes:
            t = lw.get(k)
            if t is not None and t.src != e:
                self._wait(e, t)
            r = rd.get(k)
            if r:
                for s, t in r.items():
                    if s != e:
                        self._wait(e, t)
        for t in extra:
            if t is not None:
                self._wait(e, t)

    def _commit(self, tok, reads, writes):
        rd = self.rd
        for k in reads:
            d = rd.get(k)
            if d is None:
                rd[k] = {tok.src: tok}
            else:
                d[tok.src] = tok
        for k in writes:
            self.lw[k] = tok
            rd[k] = {}

    def op(self, e, fn, reads=(), writes=(), extra=()):
        self._deps(e, reads, writes, extra)
        lst = self.ops.setdefault(e, [])
        rec = ["op", _eager(fn), False]
        lst.append(rec)
        self.q[e].append(rec)
        tok = Tok(e, len(lst))
        self._commit(tok, reads, writes)
        return tok

    def dma(self, e, sem, fn, reads=(), writes=(), extra=()):
        self._deps(e, reads, writes, extra)
        self.dma_sems.add(sem)
        lst = self.ops.setdefault(sem, [])
        rec = ["dma", _eager(fn), True, sem]
        lst.append(rec)
        self.q[e].append(rec)
        tok = Tok(sem, len(lst))
        self._commit(tok, reads, writes)
        return tok

    def collective(self, sem, fn, reads=(), writes=()):
        self._deps("pool", reads, writes, ())
        self.cc_sems.add(sem)
        rec = ["cc", _eager(fn), True, sem]
        self.ops.setdefault(sem, []).append(rec)
        self.q["pool"].append(rec)
        tok = Tok(sem, 1)
        self._commit(tok, reads, writes)
        self._wait("pool", tok)
        return tok

    def wait_all(self, e, toks):
        for t in toks:
            self._wait(e, t)

    def replay(self, nc, es):
        names = list(self.ENGS[:4]) + sorted(self.dma_sems) + sorted(self.cc_sems)
        sems = {n: es.enter_context(nc.semaphore("s_" + n)) for n in names}
        semval = {}
        for src, lst in self.ops.items():
            c = 0
            vals = []
            for rec in lst:
                if rec[2]:
                    c += 1
                vals.append(c)
            semval[src] = vals
        block = es.enter_context(nc.Block())
        engmap = {"pe": block.tensor, "act": block.scalar, "dve": block.vector, "pool": block.gpsimd, "sp": block.sync}

        def make(e):
            items = self.q[e]

            def body(eng):
                for it in items:
                    if it[0] == "wait":
                        src, idx = it[1], it[2]
                        mult = 16 if src in self.dma_sems else 1
                        eng.wait_ge(sems[src], semval[src][idx - 1] * mult)
                    elif it[0] == "op":
                        ins = it[1](eng)
                        if it[2]:
                            ins.then_inc(sems[e], 1)
                    elif it[0] == "cc":
                        it[1](eng).then_inc(sems[it[3]])
                    else:
                        it[1](eng).then_inc(sems[it[3]], 16)
            return body

        for e in self.ENGS:
            if self.q[e]:
                engmap[e](make(e))


def kchunk(name, c, tis):
    return [(name, c, t) for t in tis]


class MK:
    def __init__(self, nc, es, dbg=None, stop=None):
        self.nc = nc
        self.es = es
        self.b = Builder()
        self.dbg = dbg or []
        self.stop = stop
        self.psn = 0
        self.wsn = 0
        self._declare()

    def _dram_in(self, name, shape):
        return self.nc.dram_tensor(name, list(shape), F32, kind="ExternalInput").ap()

    def _sb(self, name, shape, dt):
        return self.es.enter_context(self.nc.sbuf_tensor(name, list(shape), dt))

    def _declare(self):
        nc = self.nc
        d = self._dram_in
        self.d_x = d("xT", [D, T])
        self.d_xp = d("xTp", [D, T])
        self.d_cst = d("cst", [128, NC])
        self.d_pc = d("pc", [128, NPC])
        self.d_smP = d("smP", [128, NSP])
        self.d_smB = d("smB", [L, 128, NSB])
        self.d_wfm = d("win_fm", [L, 128, 8, 1280])
        self.d_wkv = d("win_kv", [L, 128, 8, 384])
        self.d_wglrT = d("win_glrT", [L, 16, 1024])
        self.d_wg2 = d("wg2", [L, 16, 128])
        self.d_wrv = d("win_rv", [L, 128, 8, 512])
        self.d_wqk = d("win_qk", [L, 128, 8, 256])
        self.d_wg = d("win_g", [L, 128, 8, 4096])
        self.d_bproj = d("bproj", [L, 128, 8, 1024])
        self.d_wout = d("wout", [L, 128, 8, 1024])
        small = self.stop is not None and self.stop != "ffn1"
        self.d_f13 = d("ffn_w13", [11, 128, 8, 512])
        self.d_f2 = d("ffn_w2t", [2, 4, 128, 12, 256])
        self.d_m13 = d("moe_w13", [1, 1, 1, 1, 8] if small else [NE, 11, 128, 8, 512])
        self.d_m2 = d("moe_w2t", [1, 1, 1, 1, 1, 8] if small else [NE, 2, 4, 128, 12, 256])
        self.d_out = nc.dram_tensor("outT", [D, T], F32, kind="ExternalOutput").ap()
        self.d_dbg = {}
        for name in self.dbg:
            self.d_dbg[name] = nc.dram_tensor("dbg_" + name, [128, 8, T], F32, kind="ExternalOutput").ap()

        self.X = self._sb("X", [128, 8, T], F32)
        self.H = self._sb("H", [128, 8, T], BF16)
        self.YM = self._sb("YM", [128, 16, T], BF16)
        self.WS = self._sb("WS", [128, NSLOT, SLOT], BF16)
        self.cst = self._sb("cstS", [128, NC], F32)
        self.cstb = self._sb("cstB", [128, 392], BF16)
        self.pc = self._sb("pcS", [128, NPC], F32)
        self.smP = self._sb("smPS", [128, NSP], F32)
        self.RS = [self._sb("RS%d" % i, [128, 512], F32) for i in range(2)]
        self.Tm = [self._sb("Tm%d" % i, [128, 528], F32) for i in range(4)]
        self.S = self._sb("S", [128, 256], F32)
        self.dec = self._sb("dec", [128, 32], F32)
        self.pay = [self._sb("pay%d" % i, [128, XW], F32) for i in range(2)]
        self.xin = self._sb("xinS", [128, XW], F32)
        self.st4 = self._sb("st4", [128, 32], F32)
        self.st5 = self._sb("st5", [128, 32], F32)
        self.comb = self._sb("comb", [128, NT, 8], F32)
        self.PS = [self.es.enter_context(nc.psum_tensor("ps%d" % i, [128, 512], F32)) for i in range(8)]

    def ymflat(self):
        return self.YM[:].rearrange("p c t -> p (c t)")

    def ymkeys(self, off, n):
        ks = []
        e = (off // 128) * 128
        while e < off + n:
            ks.append(("Y", e // T, (e % T) // 128))
            e += 128
        return ks

    def ymv(self, off, n, dt=BF16):
        if dt == BF16:
            return self.ymflat()[:, off:off + n], self.ymkeys(off, n)
        v = self.ymflat()[:, off:off + 2 * n].bitcast(F32)
        return v, self.ymkeys(off, 2 * n)

    def ps(self):
        i = self.psn % 8
        self.psn += 1
        return i

    def c(self, off, n):
        return self.cst[:, off:off + n]

    def wload(self, src_ap, shape_kc_n, extra_reads=()):
        s = self.wsn % NSLOT
        self.wsn += 1
        kc, n = shape_kc_n
        assert kc * n <= SLOT
        view = self.WS[:, s, 0:kc * n].rearrange("p (k n) -> p k n", k=kc)
        self.b.dma("pool", "w%d" % s, lambda g, o=view, i=src_ap: g.dma_start(out=o, in_=i),
                   reads=(), writes=[("W", s)])
        return view, s

    def mm(self, psi, out_ap, pairs, reads, first_start=True, last_stop=True):
        n = len(pairs)
        tok = None
        for i, (lt, rh) in enumerate(pairs):
            st = first_start and i == 0
            sp = last_stop and i == n - 1
            tok = self.b.op("pe", lambda pe, o=out_ap, a=lt, r=rh, st=st, sp=sp: pe.matmul(o, a, r, start=st, stop=sp),
                            reads=reads if i == 0 else (), writes=[("ps", psi)])
        self.b._commit(tok, reads, [])
        return tok

    def dve(self, fn, reads, writes):
        return self.b.op("dve", fn, reads, writes)

    def act(self, fn, reads, writes):
        return self.b.op("act", fn, reads, writes)

    def pool(self, fn, reads, writes):
        return self.b.op("pool", fn, reads, writes)

    def dump(self, name, src_fn, keys_fn):
        if name not in self.d_dbg:
            return
        for kc in range(8):
            self.b.dma("pool", "dbg", lambda g, o=self.d_dbg[name][:, kc, :], i=src_fn(kc): g.dma_start(out=o, in_=i),
                       reads=keys_fn(kc), writes=[("dbg", name, kc)])

    def kX(self, kc, b):
        return [("X", kc, 4 * b + i) for i in range(4)]

    def kH(self, kc, b):
        return [("H", kc, 4 * b + i) for i in range(4)]

    def kHall(self, b):
        return [("H", kc, 4 * b + i) for kc in range(8) for i in range(4)]

    def kHtile(self, i):
        return [("H", kc, i) for kc in range(8)]

    def kY(self, c, b):
        return [("Y", c, 4 * b + i) for i in range(4)]

    def init(self):
        b = self.b
        b.dma("sp", "m0", lambda q: q.dma_start(out=self.cst[:], in_=self.d_cst), writes=["cst"])
        b.dma("sp", "m1", lambda q: q.dma_start(out=self.pc[:], in_=self.d_pc), writes=["pc"])
        b.dma("sp", "m2", lambda q: q.dma_start(out=self.smP[:], in_=self.d_smP), writes=["smP"])
        self.ones_bf = self.cstb[:, 0:128]
        self.tri2_bf = self.cstb[:, 128:256]
        self.rev2_bf = self.cstb[:, 256:384]
        self.ch2_bf = self.cstb[:, 384:386]
        self.dve(lambda v: v.tensor_copy(self.cstb[:, 0:128], self.c(OC_ONES, 128)), ["cst"], ["cstb"])
        self.dve(lambda v: v.tensor_copy(self.cstb[:, 128:384], self.c(OC_TRI2, 256)), ["cst"], ["cstb"])
        self.dve(lambda v: v.tensor_copy(self.cstb[:, 384:386], self.c(OC_CH2, 2)), ["cst"], ["cstb"])

    def load_x(self, src):
        for kc in range(8):
            self.b.dma("sp", "xin%d" % kc, lambda q, kc=kc: q.dma_start(out=self.X[:, kc, :], in_=src[kc * 128:(kc + 1) * 128, :]),
                       writes=[("X", kc, i) for i in range(NT)])

    def rmsnorm(self, gain_off, out_fn, out_keys_fn, hook=None):
        sq_off = 8 * T
        for b in range(NB):
            blk = slice(b * 512, (b + 1) * 512)
            psi = self.ps()
            for half in range(2):
                sq, sqk = self.ymv(sq_off + half * T, T)
                sq3 = sq.rearrange("p (k t) -> p k t", k=4)
                xr = [k for kc in range(4 * half, 4 * half + 4) for k in self.kX(kc, b)]
                self.act(lambda a, o=sq3, i=self.X[:, 4 * half:4 * half + 4, blk]: a.activation(out=o, in_=i, func=AF.Square),
                         xr, sqk)
                self.mm(psi, self.PS[psi][:, :], [(self.ones_bf, sq3[:, k, :]) for k in range(4)],
                        reads=["cstb"] + sqk, first_start=(half == 0), last_stop=(half == 1))
            rt = self.Tm[0][:, 0:512]
            rs = self.RS[b % 2]
            self.act(lambda a, o=rt, i=self.PS[psi][:, :]: a.activation(out=o, in_=i, func=AF.Sqrt, bias=EPS, scale=1.0 / D),
                     [("ps", psi)], ["Tm0"])
            self.dve(lambda v, o=rs[:], i=rt: v.reciprocal(out=o, in_=i), ["Tm0"], [("RS", b % 2)])
            for kc in range(8):
                self.dve(lambda v, o=out_fn(kc, b), i=self.X[:, kc, blk], g=self.smP[:, gain_off + kc:gain_off + kc + 1], r=rs[:]:
                         v.scalar_tensor_tensor(out=o, in0=i, scalar=g, in1=r, op0=ALU.mult, op1=ALU.mult),
                         self.kX(kc, b) + [("RS", b % 2), "smP"], out_keys_fn(kc, b))
            if hook is not None:
                hook(b, rs)

    MB = 8 * T
    O_SMB = MB
    O_WST = MB + 3360
    O_POOLW = MB + 3872
    O_KEND = MB + 4128
    O_VTM = MB + 6176
    O_LTM = MB + 10272
    O_QH = MB + 12320
    O_QZ = MB + 14368
    O_KINV = MB + 15392

    def mixer(self, l, mode="main"):
        b = self.b
        self.invc_off = 8 if mode == "main" else 40
        state_only = mode == "pstate"
        Hh, Y = self.H, self.YM
        self.rmsnorm(OP_GMIX + 8 * l, lambda kc, bb: Hh[:, kc, bb * 512:(bb + 1) * 512], lambda kc, bb: self.kH(kc, bb))
        if self.stop == "norm%d" % l:
            return
        smB, smBk = self.ymv(self.O_SMB, NSB, F32)
        b.dma("sp", "misc", lambda q: q.dma_start(out=smB, in_=self.d_smB[l]), writes=smBk)
        sB = lambda off, n: smB[:, off:off + n]
        wst_bf, wstk = self.ymv(self.O_WST, 512)
        poolw_bf, poolwk = self.ymv(self.O_POOLW, 256)
        self.dve(lambda v: v.tensor_tensor(out=wst_bf.rearrange("p (h t) -> p h t", h=4), in0=sB(OB_WST, 512).rearrange("p (h t) -> p h t", h=4),
                                           in1=self.c(OC_TRISGU, 128).unsqueeze(1).to_broadcast([128, 4, 128]), op=ALU.mult),
                 smBk + ["cst"], wstk)
        self.dve(lambda v: v.tensor_copy(poolw_bf, sB(OB_POOLW, 256)), smBk, poolwk)
        kend, kendk = self.ymv(self.O_KEND, 2048)
        vtm, vtmk = self.ymv(self.O_VTM, 4096)
        ltm, ltmk = self.ymv(self.O_LTM, 2048)
        kend3 = kend.rearrange("p (i k) -> p i k", i=NT)
        vtm3 = vtm.rearrange("p (i k) -> p i k", i=NT)
        ltm3 = ltm.rearrange("p (i k) -> p i k", i=NT)
        kendk_i = lambda i: self.ymkeys(self.O_KEND + i * 128, 128)
        vtmk_i = lambda i: self.ymkeys(self.O_VTM + i * 256, 256)
        ltmk_i = lambda i: self.ymkeys(self.O_LTM + i * 128, 128)

        for g in ((0, 2) if state_only else (0, 1, 2)):
            ncol = 512 if g < 2 else 256
            W, s = self.wload(self.d_wfm[l][:, :, g * 512:g * 512 + ncol], (8, ncol))
            for bb in ((NB - 1,) if state_only else range(NB)):
                blk = slice(bb * 512, (bb + 1) * 512)
                pss = []
                for ct in range(ncol // 128):
                    psi = self.ps()
                    self.mm(psi, self.PS[psi][:, :], [(W[:, kc, ct * 128:(ct + 1) * 128], Hh[:, kc, blk]) for kc in range(8)],
                            reads=[("W", s)] + self.kHall(bb))
                    pss.append(psi)
                if g == 0:
                    for cc in range(2):
                        tm = self.Tm[cc][:, 0:512]
                        self.act(lambda a, o=tm, i=self.PS[pss[cc]][:, :]: a.copy(out=o, in_=i), [("ps", pss[cc])], ["Tm%d" % cc])
                        self.dve(lambda v, o=Y[:, 6 + cc, blk], a_=tm, p=self.PS[pss[2 + cc]][:, :]: v.tensor_tensor(out=o, in0=p, in1=a_, op=ALU.mult),
                                 ["Tm%d" % cc, ("ps", pss[2 + cc])], self.kY(6 + cc, bb))
                elif g == 1:
                    for cc in range(2):
                        self.act(lambda a, o=Y[:, cc, blk], i=self.PS[pss[cc]][:, :]: a.activation(out=o, in_=i, func=AF.Gelu_apprx_tanh),
                                 [("ps", pss[cc])], self.kY(cc, bb))
                        self.dve(lambda v, o=Y[:, 2 + cc, blk], i=self.PS[pss[2 + cc]][:, :]: v.tensor_copy(o, i),
                                 [("ps", pss[2 + cc])], self.kY(2 + cc, bb))
                else:
                    for cc in range(2):
                        self.dve(lambda v, o=Y[:, 4 + cc, blk], i=self.PS[pss[cc]][:, :]: v.tensor_copy(o, i),
                                 [("ps", pss[cc])], self.kY(4 + cc, bb))

        if self.stop == "a1_%d" % l:
            self.dump("Y%d" % l, lambda kc: Y[:, kc, :], lambda kc: [("Y", kc, i) for i in range(NT)])
            return
        tmpw, tmpwk = self.ymv(self.O_QH, 2048)
        wglrT_bf = tmpw[0:16, 0:1024]
        wg2_bf = tmpw[0:16, 1024:1152]
        b.dma("pool", "misc2", lambda g_: g_.dma_start(out=wglrT_bf, in_=self.d_wglrT[l]), writes=tmpwk)
        b.dma("pool", "misc2", lambda g_: g_.dma_start(out=wg2_bf, in_=self.d_wg2[l]), writes=tmpwk)
        s = self.wsn % NSLOT
        self.wsn += 1
        Wkv = self.WS[:, s, 0:4096].rearrange("p (k n) -> p k n", k=8)
        b.dma("pool", "w%d" % s, lambda g_: g_.dma_start(out=Wkv[:, :, 0:384], in_=self.d_wkv[l]), writes=[("W", s)])
        for half in range(2):
            psi = self.ps()
            for k4 in range(4):
                kc = half * 4 + k4
                self.mm(psi, self.PS[psi][:, k4 * 128:(k4 + 1) * 128], [(wglrT_bf[:, kc * 128:(kc + 1) * 128], wg2_bf)], reads=tmpwk)
            self.dve(lambda v, o=Wkv[:, half * 4:half * 4 + 4, 384:512], i=self.PS[psi][:, :].rearrange("p (k n) -> p k n", k=4): v.tensor_copy(o, i),
                     [("ps", psi)], [("W", s)])
        self.pool(lambda g_: g_.memset(self.S[:], 0.0), [], ["S"])
        for i in range(NT):
            tok = slice(i * 128, (i + 1) * 128)
            psA = self.ps()
            self.mm(psA, self.PS[psA][:, :], [(Hh[:, kc, tok], Wkv[:, kc, :]) for kc in range(8)], reads=[("W", s)] + self.kHtile(i))
            PA = self.PS[psA]
            t0 = self.Tm[2 * (i % 2)]
            t1 = self.Tm[2 * (i % 2) + 1]
            k0 = "Tm%d" % (2 * (i % 2))
            k1 = "Tm%d" % (2 * (i % 2) + 1)
            self.dve(lambda v, o=t0[:, 0:128], p=PA[:, 384:512]: v.tensor_tensor(out=o, in0=p, in1=sB(OB_BG, 128), op=ALU.add),
                     [("ps", psA)] + smBk, [k0])
            self.act(lambda a, o=t0[:, 128:256], i_=t0[:, 0:128]: a.activation(out=o, in_=i_, func=AF.Exp, scale=-1.0), [k0], [k0])
            self.act(lambda a, o=t0[:, 256:384], i_=t0[:, 128:256]: a.activation(out=o, in_=i_, func=AF.Ln, bias=1.0), [k0], [k0])
            self.dve(lambda v, o=ltm3[:, i, :], i_=t0[:, 256:384]: v.tensor_copy(o, i_), [k0], ltmk_i(i))
            psB = self.ps()
            PB = self.PS[psB]
            self.mm(psB, PB[:, 0:128], [(self.rev2_bf, ltm3[:, i, :])], reads=["cstb"] + ltmk_i(i))
            self.mm(psB, PB[:, 128:130], [(ltm3[:, i, :], self.ch2_bf)], reads=["cstb"] + ltmk_i(i))
            self.act(lambda a, o=t1[:, 0:128], i_=PB[:, 0:128]: a.activation(out=o, in_=i_, func=AF.Exp), [("ps", psB)], [k1])
            self.act(lambda a, o=self.dec[:, 2 * i:2 * i + 2], i_=PB[:, 128:130]: a.activation(out=o, in_=i_, func=AF.Exp), [("ps", psB)], [("dec", i)])
            self.dve(lambda v, o=kend3[:, i, :], p=PA[:, 0:128], e=t1[:, 0:128]: v.tensor_tensor(out=o, in0=p, in1=e, op=ALU.mult),
                     [("ps", psA), k1], kendk_i(i))
            self.dve(lambda v, o=vtm3[:, i, :], p=PA[:, 128:384]: v.tensor_copy(o, p), [("ps", psA)], vtmk_i(i))
            psK = self.ps()
            for j in range(2):
                rows = slice(64 * j, 64 * j + 64)
                self.mm(psK, self.PS[psK][:, j * 256:(j + 1) * 256], [(kend3[rows, i, :], vtm3[rows, i, :])], reads=kendk_i(i) + vtmk_i(i))
                self.dve(lambda v, p=self.PS[psK][:, j * 256:(j + 1) * 256], d_=self.dec[:, 2 * i + j:2 * i + j + 1]:
                         v.scalar_tensor_tensor(out=self.S[:], in0=self.S[:], scalar=d_, in1=p, op0=ALU.mult, op1=ALU.add),
                         [("ps", psK), ("dec", i), "S"], ["S"])
        if self.stop == "a2_%d" % l and mode == "main":
            return
        if mode != "main":
            pay = self.pay[l]
            pk = ("pay", l)
            t2 = self.Tm[2]
            self.dve(lambda v: v.tensor_tensor(out=t2[:, 0:256], in0=self.S[:], in1=self.c(OC_BMASK, 256), op=ALU.mult), ["S", "cst"], ["Tm2"])
            self.dve(lambda v: v.tensor_reduce(out=pay[:, 0:64], in_=t2[:, 0:256].rearrange("p (h v) -> p v h", h=4), axis=AX.X, op=ALU.add),
                     ["Tm2"], [pk])
            self.dve(lambda v: v.tensor_copy(pay[:, 64:96].rearrange("p (c t) -> p c t", c=2), Y[:, 4:6, T - 16:T]),
                     [("Y", 4, 15), ("Y", 5, 15)], [pk])
            self.dve(lambda v: v.tensor_copy(pay[:, 96:100].rearrange("p (c t) -> p c t", c=2), Y[:, 6:8, T - 2:T]),
                     [("Y", 6, 15), ("Y", 7, 15)], [pk])
            if state_only:
                return
            self.pool(lambda g_: g_.memset(self.xin[:], 0.0), [], ["xinS"])
        else:
            self.dve(lambda v: v.tensor_scalar(out=self.xin[:], in0=self.pay[l][:], scalar1=self.pc[:, 0:1], scalar2=None, op0=ALU.mult),
                     [("pay", l), "pc"], ["xinS"])
        self.dve(lambda v: v.tensor_tensor(out=self.S[:].rearrange("p (h v) -> p h v", h=4), in0=self.c(OC_BMASK, 256).rearrange("p (h v) -> p h v", h=4),
                                           in1=self.xin[:, 0:64].unsqueeze(1).to_broadcast([128, 4, 64]), op=ALU.mult),
                 ["xinS", "cst", "S"], ["S"])
        if self.stop == "x_%d" % l:
            return
        self._mixer_b(l, smB, smBk, poolw_bf, poolwk)
        if self.stop == "b_%d" % l:
            self.dump("Y%d" % l, lambda kc: Y[:, kc, :], lambda kc: [("Y", kc, i) for i in range(NT)])
            return
        self._mixer_c(l, smB, smBk, wst_bf, wstk, kend3, kendk_i, vtm3, vtmk_i, ltm3, ltmk_i)
        self.dump("Y%d" % l, lambda kc: Y[:, kc, :], lambda kc: [("Y", kc, i) for i in range(NT)])
        if self.stop == "branch%d" % l:
            return
        self._mixer_gate(l)

    def _mixer_b(self, l, smB, smBk, poolw_bf, poolwk):
        Y = self.YM
        sB = lambda off, n: smB[:, off:off + n]
        for bb in range(NB):
            t0 = bb * 512
            blk = slice(t0, t0 + 512)
            for cc in range(2):
                ta, tb = self.Tm[2 * cc], self.Tm[2 * cc + 1]
                ka, kb_ = "Tm%d" % (2 * cc), "Tm%d" % (2 * cc + 1)
                if bb == 0:
                    self.act(lambda a, o=ta[:, 2:514], i=Y[:, 6 + cc, 0:512]: a.copy(out=o, in_=i), self.kY(6 + cc, 0), [ka])
                    self.act(lambda a, o=ta[:, 0:2], i=self.xin[:, 96 + 2 * cc:98 + 2 * cc]: a.copy(out=o, in_=i), ["xinS"], [ka])
                else:
                    self.act(lambda a, o=ta[:, 0:514], i=Y[:, 6 + cc, t0 - 2:t0 + 512]: a.copy(out=o, in_=i),
                             self.kY(6 + cc, bb) + [("Y", 6 + cc, 4 * bb - 1)], [ka])
                cw = lambda k: sB(OB_CONVW + 2 * k + cc, 1)
                self.act(lambda a, o=tb[:, 0:512], i=ta[:, 2:514]: a.activation(out=o, in_=i, func=AF.Identity, bias=sB(OB_CONVB + cc, 1), scale=cw(2)),
                         [ka] + smBk, [kb_])
                self.dve(lambda v, o=tb[:, 0:512], i=ta[:, 1:513]: v.scalar_tensor_tensor(out=o, in0=i, scalar=cw(1), in1=o, op0=ALU.mult, op1=ALU.add),
                         [ka, kb_] + smBk, [kb_])
                self.dve(lambda v, o=tb[:, 0:512], i=ta[:, 0:512]: v.scalar_tensor_tensor(out=o, in0=i, scalar=cw(0), in1=o, op0=ALU.mult, op1=ALU.add),
                         [ka, kb_] + smBk, [kb_])
                self.dve(lambda v, o=Y[:, 2 + cc, blk], i=tb[:, 0:512]: v.tensor_tensor(out=o, in0=i, in1=o, op=ALU.mult),
                         [kb_] + self.kY(2 + cc, bb), self.kY(2 + cc, bb))
        pb_all = self.RS[1][:].bitcast(BF16)
        for bb in range(NB - 1, -1, -1):
            t0 = bb * 512
            blk = slice(t0, t0 + 512)
            for cc in range(2):
                Z, s2, s4, s8 = self.Tm[0], self.Tm[1], self.Tm[2], self.Tm[3]
                if bb == 0:
                    self.act(lambda a, o=Z[:, 16:528], i=Y[:, 4 + cc, 0:512]: a.copy(out=o, in_=i), self.kY(4 + cc, 0), ["Tm0"])
                    self.act(lambda a, o=Z[:, 0:16], i=self.xin[:, 64 + 16 * cc:80 + 16 * cc]: a.copy(out=o, in_=i), ["xinS"], ["Tm0"])
                else:
                    self.act(lambda a, o=Z[:, 0:528], i=Y[:, 4 + cc, t0 - 16:t0 + 512]: a.copy(out=o, in_=i),
                             self.kY(4 + cc, bb) + [("Y", 4 + cc, 4 * bb - 1)], ["Tm0"])
                self.dve(lambda v: v.tensor_tensor(out=s2[:, 1:528], in0=Z[:, 1:528], in1=Z[:, 0:527], op=ALU.add), ["Tm0"], ["Tm1"])
                self.dve(lambda v: v.tensor_tensor(out=s4[:, 3:528], in0=s2[:, 3:528], in1=s2[:, 1:526], op=ALU.add), ["Tm1"], ["Tm2"])
                if cc == 0:
                    lo, hi, klo, khi = s2, s4, "Tm1", "Tm2"
                else:
                    self.dve(lambda v: v.tensor_tensor(out=s8[:, 7:528], in0=s4[:, 7:528], in1=s4[:, 3:524], op=ALU.add), ["Tm2"], ["Tm3"])
                    self.dve(lambda v: v.tensor_tensor(out=s2[:, 15:528], in0=s8[:, 15:528], in1=s8[:, 7:520], op=ALU.add), ["Tm3"], ["Tm1"])
                    lo, hi, klo, khi = s8, s2, "Tm3", "Tm1"
                pb = pb_all[:, cc * 512:(cc + 1) * 512]
                pbk = ("PB", cc)
                for (lev, klev, rows) in ((lo, klo, slice(0, 64)), (hi, khi, slice(64, 128))):
                    self.dve(lambda v, lev=lev, rows=rows: v.scalar_tensor_tensor(out=pb[rows, :], in0=lev[rows, 16:528], scalar=sB(OB_INVW + cc, 1)[rows, :],
                                                                                 in1=Z[rows, 16:528], op0=ALU.mult, op1=ALU.subtract),
                             [klev, "Tm0", ("RS", 1)] + smBk, [pbk, ("RS", 1)])
                    if bb == 0:
                        tq = self.st4[:, 0:16]
                        self.dve(lambda v, lev=lev, rows=rows: v.tensor_tensor(out=tq[rows, :], in0=lev[rows, 16:32], in1=self.pc[rows, self.invc_off + 16 * cc:self.invc_off + 16 + 16 * cc], op=ALU.mult),
                                 [klev, "pc"], ["st4"])
                        self.dve(lambda v, rows=rows: v.tensor_tensor(out=pb[rows, 0:16], in0=tq[rows, :], in1=Z[rows, 16:32], op=ALU.subtract),
                                 ["st4", "Tm0", pbk], [pbk])
                psi = self.ps()
                self.mm(psi, self.PS[psi][:, :], [(poolw_bf[:, cc * 128:(cc + 1) * 128], pb)], reads=poolwk + [pbk])
                self.act(lambda a, o=Y[:, 4 + cc, blk], i=self.PS[psi][:, :]: a.activation(out=o, in_=i, func=AF.Identity, scale=sB(OB_PSC + cc, 1)),
                         [("ps", psi)] + smBk, self.kY(4 + cc, bb))

    def _mixer_c(self, l, smB, smBk, wst_bf, wstk, kend3, kendk_i, vtm3, vtmk_i, ltm3, ltmk_i):
        b = self.b
        Y, Hh = self.YM, self.H
        sB = lambda off, n: smB[:, off:off + n]
        Wrv, s_rv = self.wload(self.d_wrv[l], (8, 512))
        Wqk, s_qk = self.wload(self.d_wqk[l], (8, 256))
        qh, qhk = self.ymv(self.O_QH, 2048)
        qz, qzk = self.ymv(self.O_QZ, 1024)
        kinv, kinvk = self.ymv(self.O_KINV, 512)
        qh3 = qh.rearrange("p (h t) -> p h t", h=4)
        qz4 = qz.rearrange("p (i j t) -> p i j t", i=4, j=2)
        scm_all = self.RS[0][:].bitcast(BF16)
        wst3 = wst_bf.rearrange("p (h t) -> p h t", h=4)
        s_free = self.wsn % NSLOT
        ext = self.WS[:, s_free, :].bitcast(F32)
        KW = ("W", s_free)
        self.pool(lambda g_: g_.memset(qz, 0.0), [], qzk)
        for bb in range(NB):
            blk = slice(bb * 512, (bb + 1) * 512)
            psQ, psK, psC = self.ps(), self.ps(), self.ps()
            self.mm(psQ, self.PS[psQ][:, :], [(Wqk[:, kc, 0:128], Hh[:, kc, blk]) for kc in range(8)], reads=[("W", s_qk)] + self.kHall(bb))
            self.mm(psK, self.PS[psK][:, :], [(Wqk[:, kc, 128:256], Hh[:, kc, blk]) for kc in range(8)], reads=[("W", s_qk)] + self.kHall(bb))
            for ti in range(4):
                i = 4 * bb + ti
                self.mm(psC, self.PS[psC][:, ti * 128:(ti + 1) * 128], [(ltm3[:, i, :], self.tri2_bf)], reads=["cstb"] + ltmk_i(i))
            ec, en = self.Tm[0][:, 0:512], self.Tm[1][:, 0:512]
            self.act(lambda a, i_=self.PS[psC][:, :]: a.activation(out=ec, in_=i_, func=AF.Exp), [("ps", psC)], ["Tm0"])
            self.act(lambda a, i_=self.PS[psC][:, :]: a.activation(out=en, in_=i_, func=AF.Exp, scale=-1.0), [("ps", psC)], ["Tm1"])
            for h in range(4):
                self.dve(lambda v, h=h, p=self.PS[psQ][:, :]: v.scalar_tensor_tensor(out=qh3[:, h, :], in0=p, scalar=self.c(OC_HM + h, 1), in1=ec,
                                                                                  op0=ALU.mult, op1=ALU.mult), [("ps", psQ), "Tm0", "cst"], qhk)
            self.dve(lambda v, p=self.PS[psK][:, :]: v.tensor_tensor(out=kinv, in0=p, in1=en, op=ALU.mult), [("ps", psK), "Tm1"], kinvk)
            for j in range(2):
                self.dve(lambda v, j=j, p=self.PS[psQ][:, :]: v.scalar_tensor_tensor(
                    out=qz4[:, :, j, 64 * j:64 * j + 64], in0=p.rearrange("p (i j t) -> p i j t", i=4, j=2)[:, :, j, :], scalar=float(32 ** -0.5),
                    in1=ec.rearrange("p (i j t) -> p i j t", i=4, j=2)[:, :, j, :], op0=ALU.mult, op1=ALU.mult), [("ps", psQ), "Tm0"], qzk)
            for ti in range(4):
                i = 4 * bb + ti
                tok = slice(i * 128, (i + 1) * 128)
                tl = slice(ti * 128, (ti + 1) * 128)
                psR = self.ps()
                PR = self.PS[psR]
                self.mm(psR, PR[:, :], [(Hh[:, kc, tok], Wrv[:, kc, :]) for kc in range(8)], reads=[("W", s_rv)] + self.kHtile(i))
                par = i % 2
                if par == 0:
                    t2, t3a, sb_all, wide, st = self.Tm[2][:, 0:512], self.Tm[3][:, 0:256], self.Tm[3][:, 256:512].bitcast(BF16), self.RS[1][:, :], self.st4
                    XK, XW_ = [], []
                else:
                    t2, t3a, sb_all, wide, st = ext[:, 0:512], ext[:, 512:768], ext[:, 768:1024].bitcast(BF16), ext[:, 1024:1536], self.st5
                    XK, XW_ = [KW], [KW]
                kT2, kT2b, kT3, kSt, kStg, kWA, kWB, kRS1 = ("Tm2%d" % par, "Tm2b%d" % par, "Tm3%d" % par, "st4%d" % par, "st4g%d" % par, "wA%d" % par, "wB%d" % par, ("RS", 1) if par == 0 else KW)
                if par == 0:
                    kT2, kT2b, kT3, kSt = "Tm2", "Tm2b", "Tm3", "st4"
                vg, sq = t2[:, 0:256], t2[:, 256:512]
                self.act(lambda a: a.activation(out=vg, in_=PR[:, 256:512], func=AF.Gelu_apprx_tanh), [("ps", psR)], [kT2] + XW_)
                self.act(lambda a: a.activation(out=sq, in_=vg, func=AF.Square), [kT2] + XK, [kT2b])
                self.dve(lambda v: v.tensor_reduce(out=st[:, 0:4], in_=sq.rearrange("p (h d) -> p h d", h=4), axis=AX.X, op=ALU.add), [kT2b] + XK, [kSt])
                self.act(lambda a: a.activation(out=st[:, 4:8], in_=st[:, 0:4], func=AF.Sqrt, bias=EPS, scale=1.0 / 64), [kSt], [kSt])
                self.dve(lambda v: v.reciprocal(out=st[:, 8:12], in_=st[:, 4:8]), [kSt], [kSt])
                self.dve(lambda v: v.tensor_tensor(out=vg.rearrange("p (h d) -> p h d", h=4), in0=vg.rearrange("p (h d) -> p h d", h=4),
                                                   in1=st[:, 8:12].unsqueeze(2).to_broadcast([128, 4, 64]), op=ALU.mult), [kT2, kSt] + XK, [kT2])
                vnb = sq.bitcast(BF16)[:, 0:256]
                self.dve(lambda v: v.tensor_tensor(out=vnb, in0=vg, in1=sB(OB_SGUN, 256), op=ALU.mult), [kT2, kT2b] + smBk + XK, [kT2b])
                psM = self.ps()
                PM = self.PS[psM]
                for h in range(4):
                    cc, hh = h // 2, h % 2
                    self.mm(psM, PM[64 * hh:64 * hh + 64, cc * 128:(cc + 1) * 128], [(vnb[:, 64 * h:64 * h + 64], wst3[:, h, :])], reads=[kT2b] + wstk + XK)
                self.dve(lambda v: v.tensor_tensor(out=t3a, in0=PM[:, 0:256], in1=sB(OB_SGUB, 256), op=ALU.add), [("ps", psM)] + smBk + XK, [kT3])
                self.dve(lambda v, tok=tok: v.tensor_tensor(out=Y[:, 0:2, tok], in0=t3a.rearrange("p (c t) -> p c t", c=2), in1=Y[:, 0:2, tok], op=ALU.mult),
                         [kT3, ("Y", 0, i), ("Y", 1, i)] + XK, [("Y", 0, i), ("Y", 1, i)])
                psV = self.ps()
                for j in range(2):
                    rows = slice(64 * j, 64 * j + 64)
                    sbj = sb_all[:, j * 256:(j + 1) * 256]
                    self.dve(lambda v, sbj=sbj: v.tensor_tensor(out=sbj, in0=self.S[:], in1=self.c(OC_BMASK, 256), op=ALU.mult), ["S", "cst"] + XK, [("Sb", j, par)])
                    self.mm(psV, self.PS[psV][:, j * 256:(j + 1) * 256], [(kend3[rows, i, :], vtm3[rows, i, :])], reads=kendk_i(i) + vtmk_i(i))
                    self.dve(lambda v, p=self.PS[psV][:, j * 256:(j + 1) * 256], d_=self.dec[:, 2 * i + j:2 * i + j + 1]:
                             v.scalar_tensor_tensor(out=self.S[:], in0=self.S[:], scalar=d_, in1=p, op0=ALU.mult, op1=ALU.add),
                             [("ps", psV), ("dec", i), "S"], ["S"])
                psS = self.ps()
                PSs = self.PS[psS]
                for h in range(4):
                    self.mm(psS, PSs[:, h * 128:(h + 1) * 128], [(kinv[:, tl], qh3[:, h, tl])], reads=kinvk + qhk)
                scm = scm_all[:, (i % 2) * 512:(i % 2) * 512 + 512]
                scmk = ("scm", i % 2)
                self.dve(lambda v, scm=scm: v.tensor_tensor(out=scm.rearrange("p (h t) -> p h t", h=4), in0=PSs[:, :].rearrange("p (h t) -> p h t", h=4),
                                                           in1=self.c(OC_CAUS2, 128).unsqueeze(1).to_broadcast([128, 4, 128]), op=ALU.mult),
                         [("ps", psS), "cst", ("RS", 0)], [scmk, ("RS", 0)])
                psO = self.ps()
                PO = self.PS[psO]
                pairs_reads = [("Sb", 0, par), ("Sb", 1, par), scmk] + qzk + vtmk_i(i) + XK
                self.b.op("pe", lambda pe: pe.matmul(PO[:, 0:256], qz4[:, ti, 0, :], sb_all[:, 0:256], start=True, stop=False),
                          reads=pairs_reads, writes=[("ps", psO)])
                self.b.op("pe", lambda pe: pe.matmul(PO[:, 0:256], qz4[:, ti, 1, :], sb_all[:, 256:512], start=False, stop=False),
                          reads=(), writes=[("ps", psO)])
                tk = None
                for h in range(4):
                    tk = self.b.op("pe", lambda pe, h=h, scm=scm: pe.matmul(PO[:, 64 * h:64 * h + 64], scm[:, h * 128:(h + 1) * 128], vtm3[:, i, 64 * h:64 * h + 64],
                                                                          start=False, stop=(h == 3)), reads=(), writes=[("ps", psO)])
                self.b._commit(tk, pairs_reads, [])
                osq, sr = wide[:, 0:256], wide[:, 256:512]
                self.act(lambda a: a.activation(out=osq, in_=PO[:, 0:256], func=AF.Square), [("ps", psO)] + XK, [kWA] + ([("RS", 1)] if par == 0 else []))
                self.dve(lambda v: v.tensor_reduce(out=st[:, 16:20], in_=osq.rearrange("p (h d) -> p h d", h=4), axis=AX.X, op=ALU.add), [kWA] + XK, [kStg])
                self.act(lambda a: a.activation(out=st[:, 20:24], in_=st[:, 16:20], func=AF.Sqrt, bias=EPS, scale=1.0 / 64), [kStg], [kStg])
                self.dve(lambda v: v.reciprocal(out=st[:, 24:28], in_=st[:, 20:24]), [kStg], [kStg])
                self.dve(lambda v: v.tensor_tensor(out=osq.rearrange("p (h d) -> p h d", h=4), in0=PO[:, 0:256].rearrange("p (h d) -> p h d", h=4),
                                                   in1=st[:, 24:28].unsqueeze(2).to_broadcast([128, 4, 64]), op=ALU.mult), [("ps", psO), kStg, kWA] + XK, [kWA])
                self.dve(lambda v: v.tensor_tensor(out=osq, in0=osq, in1=sB(OB_GLAN, 256), op=ALU.mult), [kWA] + smBk + XK, [kWA])
                self.act(lambda a: a.activation(out=sr, in_=PR[:, 0:256], func=AF.Silu), [("ps", psR)] + XK, [kWB] + ([("RS", 1)] if par == 0 else []))
                self.dve(lambda v: v.tensor_tensor(out=osq, in0=osq, in1=sr, op=ALU.mult), [kWA, kWB] + XK, [kWA])
                psT = self.ps()
                PT = self.PS[psT]
                for cc in range(2):
                    self.b.op("pe", lambda pe, cc=cc: pe.transpose(PT[:, cc * 128:(cc + 1) * 128], osq[:, cc * 128:(cc + 1) * 128], self.c(OC_IDENT, 128)),
                              reads=[kWA, "cst"] + XK, writes=[("ps", psT)])
                self.act(lambda a, tok=tok: a.copy(out=Y[:, 6:8, tok], in_=PT[:, 0:256].rearrange("p (c t) -> p c t", c=2)),
                         [("ps", psT)], [("Y", 6, i), ("Y", 7, i)])

    def _mixer_gate(self, l):
        Y, Hh, X = self.YM, self.H, self.X
        for j in range(8):
            Wg, sg = self.wload(self.d_wg[l, j] if False else self.d_wg[l][:, :, j * 512:(j + 1) * 512], (8, 512))
            Pj, sp_ = self.wload(self.d_bproj[l][:, :, j * 128:(j + 1) * 128], (8, 128))
            for bb in range(NB):
                blk = slice(bb * 512, (bb + 1) * 512)
                acc, tmp = self.Tm[2][:, 0:512], self.Tm[3][:, 0:512]
                for i in range(4):
                    psG, psP = self.ps(), self.ps()
                    self.mm(psG, self.PS[psG][:, :], [(Wg[:, kc, i * 128:(i + 1) * 128], Hh[:, kc, blk]) for kc in range(8)],
                            reads=[("W", sg)] + self.kHall(bb))
                    self.mm(psP, self.PS[psP][:, :], [(Pj[:, 2 * i + c, :], Y[:, 2 * i + c, blk]) for c in range(2)],
                            reads=[("W", sp_)] + self.kY(2 * i, bb) + self.kY(2 * i + 1, bb))
                    gt = self.Tm[i % 2][:, 0:512]
                    gk = "Tm%d" % (i % 2)
                    col = OP_BGATE + 32 * l + i * 8 + j
                    self.act(lambda a, gt=gt, p=self.PS[psG][:, :], col=col: a.activation(out=gt, in_=p, func=AF.Sigmoid, bias=self.smP[:, col:col + 1]),
                             [("ps", psG), "smP"], [gk])
                    pp = self.PS[psP][:, :]
                    if i == 0:
                        self.dve(lambda v, gt=gt, pp=pp: v.tensor_tensor(out=acc, in0=pp, in1=gt, op=ALU.mult), [("ps", psP), gk], ["Tm2"])
                    else:
                        self.dve(lambda v, gt=gt, pp=pp: v.tensor_tensor(out=tmp, in0=pp, in1=gt, op=ALU.mult), [("ps", psP), gk], ["Tm3"])
                        if i < 3:
                            self.dve(lambda v: v.tensor_tensor(out=acc, in0=acc, in1=tmp, op=ALU.add), ["Tm2", "Tm3"], ["Tm2"])
                        else:
                            self.dve(lambda v, o=Y[:, 8 + j, blk]: v.tensor_tensor(out=o, in0=acc, in1=tmp, op=ALU.add), ["Tm2", "Tm3"], self.kY(8 + j, bb))
        self.dump("M%d" % l, lambda kc: Y[:, 8 + kc, :], lambda kc: [("Y", 8 + kc, i) for i in range(NT)])
        if self.stop == "gate%d" % l:
            return
        for dh in range(2):
            Wo, so = self.wload(self.d_wout[l][:, :, dh * 512:(dh + 1) * 512], (8, 512))
            for dd in range(4):
                dt = dh * 4 + dd
                for bb in range(NB):
                    blk = slice(bb * 512, (bb + 1) * 512)
                    psi = self.ps()
                    self.mm(psi, self.PS[psi][:, :], [(Wo[:, j, dd * 128:(dd + 1) * 128], Y[:, 8 + j, blk]) for j in range(8)],
                            reads=[("W", so)] + [k for j in range(8) for k in self.kY(8 + j, bb)])
                    self.dve(lambda v, o=X[:, dt, blk], p=self.PS[psi][:, :]: v.tensor_tensor(out=o, in0=p, in1=o, op=ALU.add),
                             [("ps", psi)] + self.kX(dt, bb), self.kX(dt, bb))
        self.dump("XM%d" % l, lambda kc: X[:, kc, :], lambda kc: [("X", kc, i) for i in range(NT)])

    FSPLIT = (12, 10)

    def ffn(self, d_w13, d_w2, cb=None, cbk=None):
        Y, Hh, X = self.YM, self.H, self.X
        f0 = 0
        for fh in range(2):
            nf = self.FSPLIT[fh]
            for g in range(nf // 2):
                W, s = self.wload(d_w13[f0 // 2 + g], (8, 512))
                for f2 in range(2):
                    fl = 2 * g + f2
                    for bb in range(NB):
                        blk = slice(bb * 512, (bb + 1) * 512)
                        ps1, ps3 = self.ps(), self.ps()
                        self.mm(ps1, self.PS[ps1][:, :], [(W[:, kc, f2 * 128:(f2 + 1) * 128], Hh[:, kc, blk]) for kc in range(8)],
                                reads=[("W", s)] + self.kHall(bb))
                        self.mm(ps3, self.PS[ps3][:, :], [(W[:, kc, 256 + f2 * 128:256 + (f2 + 1) * 128], Hh[:, kc, blk]) for kc in range(8)],
                                reads=[("W", s)] + self.kHall(bb))
                        n = (fl * NB + bb) % 2
                        sl = self.Tm[n][:, 0:512]
                        self.act(lambda a, sl=sl, p=self.PS[ps1][:, :]: a.activation(out=sl, in_=p, func=AF.Silu), [("ps", ps1)], ["Tm%d" % n])
                        if cb is None:
                            self.dve(lambda v, o=Y[:, fl, blk], sl=sl, p=self.PS[ps3][:, :]: v.tensor_tensor(out=o, in0=p, in1=sl, op=ALU.mult),
                                     [("ps", ps3), "Tm%d" % n], self.kY(fl, bb))
                        else:
                            t2 = self.Tm[2 + n][:, 0:512]
                            self.dve(lambda v, t2=t2, sl=sl, p=self.PS[ps3][:, :]: v.tensor_tensor(out=t2, in0=p, in1=sl, op=ALU.mult),
                                     [("ps", ps3), "Tm%d" % n], ["Tm%d" % (2 + n)])
                            self.dve(lambda v, o=Y[:, fl, blk], t2=t2, c_=cb[:, blk]: v.tensor_tensor(out=o, in0=t2, in1=c_, op=ALU.mult),
                                     ["Tm%d" % (2 + n)] + cbk(bb), self.kY(fl, bb))
            for dq in range(4):
                W2, s2 = self.wload(d_w2[fh, dq], (12, 256))
                for dd in range(2):
                    dt = dq * 2 + dd
                    for bb in range(NB):
                        blk = slice(bb * 512, (bb + 1) * 512)
                        psi = self.ps()
                        self.mm(psi, self.PS[psi][:, :], [(W2[:, f, dd * 128:(dd + 1) * 128], Y[:, f, blk]) for f in range(nf)],
                                reads=[("W", s2)] + [k for f in range(nf) for k in self.kY(f, bb)])
                        self.dve(lambda v, o=X[:, dt, blk], p=self.PS[psi][:, :]: v.tensor_tensor(out=o, in0=p, in1=o, op=ALU.add),
                                 [("ps", psi)] + self.kX(dt, bb), self.kX(dt, bb))
            f0 += nf

    def router_hook(self, gR):
        X = self.X

        def hook(bb, rs):
            for ti in range(4):
                i = 4 * bb + ti
                tok = slice(i * 128, (i + 1) * 128)
                psi = self.ps()
                P = self.PS[psi]
                self.mm(psi, P[:, 0:8], [(X[:, kc, tok], gR[:, kc * 8:(kc + 1) * 8]) for kc in range(8)],
                        reads=[("X", kc, i) for kc in range(8)] + ["gR"])
                self.b.op("pe", lambda pe, ti=ti: pe.transpose(P[:, 128:256], rs[:, ti * 128:(ti + 1) * 128], self.c(OC_IDENT, 128)),
                          reads=[("RS", bb % 2), "cst"], writes=[("ps", psi)])
                r = self.Tm[3]
                lg, eq1, l2, eq2 = r[:, 0:8], r[:, 8:16], r[:, 16:24], r[:, 24:32]
                m1, m2, dm, e2, w1, w2, rsd = (r[:, 32 + k:33 + k] for k in range(7))
                K = ["Tm3"]
                self.dve(lambda v: v.tensor_copy(lg, P[:, 0:8]), [("ps", psi)], K)
                self.dve(lambda v: v.tensor_copy(rsd, P[:, 128:129]), [("ps", psi)], K)
                self.dve(lambda v: v.reduce_max(out=m1, in_=lg, axis=AX.X), K, K)
                self.dve(lambda v: v.tensor_scalar(out=eq1, in0=lg, scalar1=m1, scalar2=None, op0=ALU.is_equal), K, K)
                self.dve(lambda v: v.scalar_tensor_tensor(out=l2, in0=eq1, scalar=-1e30, in1=lg, op0=ALU.mult, op1=ALU.add), K, K)
                self.dve(lambda v: v.reduce_max(out=m2, in_=l2, axis=AX.X), K, K)
                self.dve(lambda v: v.tensor_scalar(out=eq2, in0=l2, scalar1=m2, scalar2=None, op0=ALU.is_equal), K, K)
                self.dve(lambda v: v.tensor_tensor(out=dm, in0=m2, in1=m1, op=ALU.subtract), K, K)
                self.dve(lambda v: v.tensor_tensor(out=dm, in0=dm, in1=rsd, op=ALU.mult), K, K)
                self.act(lambda a: a.activation(out=e2, in_=dm, func=AF.Exp), K, K)
                self.dve(lambda v: v.tensor_scalar(out=w1, in0=e2, scalar1=1.0, scalar2=None, op0=ALU.add), K, K)
                self.dve(lambda v: v.reciprocal(out=w1, in_=w1), K, K)
                self.dve(lambda v: v.tensor_tensor(out=w2, in0=e2, in1=w1, op=ALU.mult), K, K)
                self.dve(lambda v, i=i: v.tensor_scalar(out=self.comb[:, i, :], in0=eq1, scalar1=w1, scalar2=None, op0=ALU.mult), K, [("comb", i)])
                self.dve(lambda v, i=i: v.scalar_tensor_tensor(out=self.comb[:, i, :], in0=eq2, scalar=w2, in1=self.comb[:, i, :], op0=ALU.mult, op1=ALU.add),
                         K + [("comb", i)], [("comb", i)])
        return hook

    def moe(self, l):
        Y = self.YM
        gR = self.Tm[2][:, 0:64]
        for kc in range(8):
            self.dve(lambda v, kc=kc: v.tensor_scalar(out=gR[:, kc * 8:(kc + 1) * 8], in0=self.smP[:, OP_ROUTER + kc * 8:OP_ROUTER + (kc + 1) * 8],
                                                      scalar1=self.smP[:, OP_GFFN + 8 * l + kc:OP_GFFN + 8 * l + kc + 1], scalar2=None, op0=ALU.mult),
                     ["smP"], ["gR", "Tm2"])
        self.rmsnorm(OP_GFFN + 8 * l, lambda kc, bb: self.H[:, kc, bb * 512:(bb + 1) * 512], lambda kc, bb: self.kH(kc, bb), hook=self.router_hook(gR))
        for e in range(NE):
            cbc = 12 + (e % 2)
            cb = Y[:, cbc, :]
            for bb in range(NB):
                psi = self.ps()
                for ti in range(4):
                    i = 4 * bb + ti
                    dg = Y[:, 14, (i % 4) * 128:(i % 4) * 128 + 128]
                    dgk = [("Y", 14, i % 4)]
                    self.dve(lambda v, dg=dg, i=i, e=e: v.tensor_scalar(out=dg, in0=self.c(OC_IDENT, 128), scalar1=self.comb[:, i, e:e + 1], scalar2=None, op0=ALU.mult),
                             ["cst", ("comb", i)], dgk)
                    self.mm(psi, self.PS[psi][:, ti * 128:(ti + 1) * 128], [(self.ones_bf, dg)], reads=["cstb"] + dgk)
                self.act(lambda a, o=cb[:, bb * 512:(bb + 1) * 512], p=self.PS[psi][:, :]: a.copy(out=o, in_=p), [("ps", psi)], self.kY(cbc, bb))
            self.ffn(self.d_m13[e], self.d_m2[e], cb=cb, cbk=lambda bb, cbc=cbc: self.kY(cbc, bb))

    def dense_ffn(self, l):
        self.rmsnorm(OP_GFFN + 8 * l, lambda kc, bb: self.H[:, kc, bb * 512:(bb + 1) * 512], lambda kc, bb: self.kH(kc, bb))
        self.ffn(self.d_f13, self.d_f2)

    def final(self):
        flat = self.ymflat()

        def out_fn(kc, bb):
            base = (bb % 2) * 8192
            return flat[:, base + kc * 1024:base + (kc + 1) * 1024].bitcast(F32)

        def out_keys(kc, bb):
            base = (bb % 2) * 8192
            return self.ymkeys(base + kc * 1024, 1024)

        def hook(bb, rs):
            for kc in range(8):
                self.b.dma("sp", "out%d_%d" % (bb % 2, kc), lambda q, kc=kc, bb=bb: q.dma_start(out=self.d_out[kc * 128:(kc + 1) * 128, bb * 512:(bb + 1) * 512], in_=out_fn(kc, bb)),
                           reads=out_keys(kc, bb), writes=[("out", kc, bb)])
        self.rmsnorm(OP_FIN, out_fn, out_keys, hook=hook)

    def finish(self):
        toks = []
        for k, t in self.b.lw.items():
            if isinstance(k, tuple) and k[0] in ("out", "dbg"):
                toks.append(t)
        self.b.wait_all("sp", toks)
        self.b.replay(self.nc, self.es)

    def program(self, skipP=False):
        self.init()
        if not skipP:
            self.load_x(self.d_xp)
            self.mixer(0, "pfull")
            self.dense_ffn(0)
            self.mixer(1, "pstate")
        else:
            for l in range(L):
                self.pool(lambda g_, l=l: g_.memset(self.pay[l][:], 0.0), [], [("pay", l)])
        self.load_x(self.d_x)
        for l in range(L):
            self.mixer(l, "main")
            if self.stop is not None and self.stop.endswith(str(l)) and self.stop[:-1] in ("norm", "branch", "gate", "mixer", "a1_", "a2_", "x_", "b_"):
                break
            if l % 2 == 0:
                self.dense_ffn(l)
            else:
                self.moe(l)
            self.dump("XF%d" % l, lambda kc: self.X[:, kc, :], lambda kc: [("X", kc, i) for i in range(NT)])
            if self.stop == "ffn%d" % l:
                break
        self.final()
        self.finish()


def _kmaj(w):
    K, N = w.shape
    return np.ascontiguousarray(w.reshape(K // 128, 128, N).transpose(1, 0, 2))


def _consts():
    p = np.arange(128)
    c = np.zeros((128, NC), np.float32)
    c[:, OC_IDENT:OC_IDENT + 128] = np.eye(128)
    s, t = p[:, None], p[None, :]
    c[:, OC_TRISGU:OC_TRISGU + 128] = (s <= t)
    same = (s // 64) == (t // 64)
    c[:, OC_CAUS2:OC_CAUS2 + 128] = same & (s <= t)
    hv = np.arange(256)[None, :]
    c[:, OC_BMASK:OC_BMASK + 256] = (p[:, None] // 32) == (hv // 64)
    c[:, OC_TRI2:OC_TRI2 + 128] = (same & (s <= t)) * (-1.0 / 16)
    c[:, OC_REV2:OC_REV2 + 128] = (same & (s > t)) * (-1.0 / 16)
    c[:, OC_CH2:OC_CH2 + 2] = ((p[:, None] // 64) == np.arange(2)[None, :]) * (-1.0 / 16)
    c[:, OC_ONES:OC_ONES + 128] = 1.0
    c[:, OC_HM:OC_HM + 4] = ((p[:, None] // 32) == np.arange(4)[None, :]) * (32 ** -0.5)
    return c


def _prep(inp, small=False):
    f = lambda a: np.ascontiguousarray(a, dtype=np.float32)
    w_in = inp["w_in"]
    out = {}
    out["cst"] = _consts()
    smP = np.zeros((128, NSP), np.float32)
    for l in range(L):
        smP[:, OP_GMIX + 8 * l:OP_GMIX + 8 * l + 8] = inp["norm_mix"][l].reshape(8, 128).T
        smP[:, OP_GFFN + 8 * l:OP_GFFN + 8 * l + 8] = inp["norm_ffn"][l].reshape(8, 128).T
        bg = inp["b_gate"][l].reshape(4, 8, 128)
        smP[:, OP_BGATE + 32 * l:OP_BGATE + 32 * l + 32] = bg.transpose(2, 0, 1).reshape(128, 32)
    smP[:, OP_FIN:OP_FIN + 8] = inp["final_norm"].reshape(8, 128).T
    smP[:, OP_ROUTER:OP_ROUTER + 64] = inp["moe_router"][0].reshape(8, 128, 8).transpose(1, 0, 2).reshape(128, 64)
    out["smP"] = smP
    smB = np.zeros((L, 128, NSB), np.float32)
    wins = (2, 4, 8, 16)
    for l in range(L):
        sb = inp["sgu_b"][l]
        smB[l, :, OB_SGUB:OB_SGUB + 256] = np.repeat(sb.reshape(2, 2, 128), 64, axis=1).transpose(1, 0, 2).reshape(128, 256)
        smB[l, :, OB_SGUN:OB_SGUN + 256] = inp["sgu_norm"][l][None, :]
        smB[l, :, OB_GLAN:OB_GLAN + 256] = np.tile(inp["gla_norm"][l], 4)[None, :]
        smB[l, :, OB_WST:OB_WST + 512] = inp["sgu_w"][l].transpose(2, 0, 1).reshape(128, 512)
        pw = np.zeros((128, 2, 128), np.float32)
        for g in range(4):
            cc, hh = g // 2, g % 2
            pw[64 * hh:64 * hh + 64, cc, 64 * hh:64 * hh + 64] = inp["pool_w"][l][g]
        smB[l, :, OB_POOLW:OB_POOLW + 256] = pw.reshape(128, 256)
        smB[l, :, OB_BG:OB_BG + 128] = inp["gla_bg"][l][None, :]
        smB[l, :, OB_CONVW:OB_CONVW + 6] = inp["conv_w"][l].reshape(3, 2, 128).transpose(2, 0, 1).reshape(128, 6)
        smB[l, :, OB_CONVB:OB_CONVB + 2] = inp["conv_b"][l].reshape(2, 128).T
        smB[l, :, OB_PSC:OB_PSC + 2] = inp["pool_scale"][l].reshape(2, 128).T
        for cc in range(2):
            for hh in range(2):
                smB[l, 64 * hh:64 * hh + 64, OB_INVW + cc] = 1.0 / wins[2 * cc + hh]
    out["smB"] = smB
    cols = lambda a, b_: list(range(a, b_))
    fm = cols(512, 768) + cols(1024, 1280) + cols(0, 256) + cols(768, 1024) + cols(1280, 1536)
    kv = cols(1664, 1792) + cols(1792, 2048)
    rv = cols(2048, 2304) + cols(256, 512)
    qk = cols(1536, 1664) + cols(1664, 1792)
    gc_ = []
    for j in range(8):
        for i in range(4):
            gc_ += cols(2320 + i * 1024 + j * 128, 2320 + i * 1024 + (j + 1) * 128)
    out["win_fm"] = np.stack([_kmaj(w_in[l][:, fm]) for l in range(L)])
    out["win_kv"] = np.stack([_kmaj(w_in[l][:, kv]) for l in range(L)])
    out["win_glrT"] = np.stack([f(w_in[l][:, 2304:2320].T) for l in range(L)])
    out["wg2"] = f(inp["gla_wg2"])
    out["win_rv"] = np.stack([_kmaj(w_in[l][:, rv]) for l in range(L)])
    out["win_qk"] = np.stack([_kmaj(w_in[l][:, qk]) for l in range(L)])
    out["win_g"] = np.stack([_kmaj(w_in[l][:, gc_]) for l in range(L)])
    out["bproj"] = np.stack([_kmaj(inp["branch_proj"][l].reshape(1024, 1024)) for l in range(L)])
    out["wout"] = np.stack([_kmaj(inp["w_out"][l]) for l in range(L)])

    def w13(w1, w3):
        a = np.empty((11, 128, 8, 512), np.float32)
        k1, k3 = _kmaj(w1), _kmaj(w3)
        for g in range(11):
            a[g, :, :, 0:256] = k1[:, :, 256 * g:256 * (g + 1)]
            a[g, :, :, 256:512] = k3[:, :, 256 * g:256 * (g + 1)]
        return a

    def w2t(w2):
        a = np.zeros((2, 4, 128, 12, 256), np.float32)
        k2 = _kmaj(w2)
        f0 = 0
        for fh, nf in enumerate((12, 10)):
            for dq in range(4):
                a[fh, dq, :, 0:nf, :] = k2[:, f0:f0 + nf, dq * 256:(dq + 1) * 256]
            f0 += nf
        return a

    out["ffn_w13"] = w13(inp["ffn_w1"][0], inp["ffn_w3"][0])
    out["ffn_w2t"] = w2t(inp["ffn_w2"][0])
    if small:
        out["moe_w13"] = np.zeros((1, 1, 1, 1, 8), np.float32)
        out["moe_w2t"] = np.zeros((1, 1, 1, 1, 1, 8), np.float32)
    else:
        out["moe_w13"] = np.stack([w13(inp["moe_w1"][0][e], inp["moe_w3"][0][e]) for e in range(NE)])
        out["moe_w2t"] = np.stack([w2t(inp["moe_w2"][0][e]) for e in range(NE)])
    return out


def _percore(x, c):
    bi, half = c // 2, c % 2
    xT = np.ascontiguousarray(x[bi, half * T:(half + 1) * T, :].T, dtype=np.float32)
    if half == 1:
        xTp = np.ascontiguousarray(x[bi, 0:T, :].T, dtype=np.float32)
    else:
        xTp = np.zeros((D, T), np.float32)
    pc = np.zeros((128, NPC), np.float32)
    pc[:, 0] = float(half)
    wins = (2, 4, 8, 16)
    t = np.arange(16)
    for cc in range(2):
        for hh in range(2):
            w = wins[2 * cc + hh]
            first = 1.0 / np.minimum(t + 1, w)
            v = first if half == 0 else np.full(16, 1.0 / w)
            pc[64 * hh:64 * hh + 64, 8 + 16 * cc:24 + 16 * cc] = v[None, :]
            pc[64 * hh:64 * hh + 64, 40 + 16 * cc:56 + 16 * cc] = first[None, :]
    return xT, xTp, pc


_NC_CACHE = {}


def build_nc(dbg=(), stop=None, skipP=False):
    key = (tuple(dbg), stop, skipP)
    if key in _NC_CACHE:
        return _NC_CACHE[key]
    nc = bass.Bass("TRN2", target_bir_lowering=False)
    es = ExitStack()
    mk = MK(nc, es, dbg=list(dbg), stop=stop)
    mk.program(skipP=skipP)
    es.close()
    _NC_CACHE[key] = nc
    return nc


def run(inputs, dbg=(), stop=None, trace=False, skipP=False):
    inp = {k: np.asarray(v) for k, v in inputs.items()}
    small = stop is not None and stop != "ffn1"
    shared = _prep(inp, small)
    in_maps = []
    for c in range(NCORES):
        xT, xTp, pc = _percore(inp["x"], c)
        m = dict(shared)
        m["xT"] = xT
        m["xTp"] = xTp
        m["pc"] = pc
        in_maps.append(m)
    nc = build_nc(dbg, stop, skipP)
    res = run_bass_kernel_spmd(nc, in_maps, core_ids=list(range(NCORES)), **({"trace": True} if trace else {}))
    return res


def kernel(**inputs):
    res = run(inputs)
    x = np.asarray(inputs["x"])
    out = np.empty(x.shape, np.float32)
    for c in range(NCORES):
        bi, half = c // 2, c % 2
        out[bi, half * T:(half + 1) * T, :] = res.results[c]["outT"].T
    return out
```

```python
import numpy as np
from contextlib import ExitStack
import concourse.bass as bass
import concourse.mybir as mybir
from concourse.bass_utils import run_bass_kernel_spmd

F32 = mybir.dt.float32
BF16 = mybir.dt.bfloat16
AF = mybir.ActivationFunctionType
ALU = mybir.AluOpType
AX = mybir.AxisListType

NCORES = 8
D = 1024
T = 2048
NT = 16
NB = 4
DFF = 2816
NE = 8
EPS = 1e-6
L = 2
NSLOT = 3
SLOT = 4096

OB_SGUB = 0
OB_SGUN = 256
OB_GLAN = 512
OB_WST = 768
OB_POOLW = 1280
OB_BG = 1536
OB_CONVW = 1664
OB_CONVB = 1670
OB_PSC = 1672
OB_INVW = 1674
NSB = 1676
OP_GMIX = 0
OP_GFFN = 16
OP_BGATE = 32
OP_FIN = 96
OP_ROUTER = 104
NSP = 168
OC_IDENT = 0
OC_TRISGU = 128
OC_CAUS2 = 256
OC_BMASK = 384
OC_TRI2 = 640
OC_REV2 = 768
OC_CH2 = 896
OC_ONES = 898
OC_HM = 1026
NC = 1030
NPC = 72
XW = 100


class Tok:
    __slots__ = ("src", "idx")

    def __init__(self, src, idx):
        self.src = src
        self.idx = idx


class _Rec:
    def __init__(self):
        self.call = None

    def __getattr__(self, name):
        def f(*a, **k):
            assert self.call is None
            self.call = (name, a, k)
            return self
        return f


def _eager(fn):
    r = _Rec()
    fn(r)
    name, a, k = r.call
    return lambda eng: getattr(eng, name)(*a, **k)


class Builder:
    ENGS = ("pe", "act", "dve", "pool", "sp")

    def __init__(self):
        self.q = {e: [] for e in self.ENGS}
        self.nops = {}
        self.ops = {}
        self.waited = {e: {} for e in self.ENGS}
        self.lw = {}
        self.rd = {}
        self.dma_sems = set()
        self.cc_sems = set()

    def _wait(self, e, tok):
        if tok.src == e and e == "pe":
            return
        w = self.waited[e]
        if w.get(tok.src, 0) >= tok.idx:
            return
        w[tok.src] = tok.idx
        self.q[e].append(["wait", tok.src, tok.idx])
        self.ops[tok.src][tok.idx - 1][2] = True

    def _deps(self, e, reads, writes, extra):
        lw, rd = self.lw, self.rd
        for k in reads:
            t = lw.get(k)
            if t is not None:
                self._wait(e, t)
        for k in writes:
            t = lw.get(k)
            if t is not None and t.src != e:
                self._wait(e, t)
            r = rd.get(k)
            if r:
                for s, t in r.items():
                    if s != e:
                        self._wait(e, t)
        for t in extra:
            if t is not None:
                self._wait(e, t)

    def _commit(self, tok, reads, writes):
        rd = self.rd
        for k in reads:
            d = rd.get(k)
            if d is None:
                rd[k] = {tok.src: tok}
            else:
                d[tok.src] = tok
        for k in writes:
            self.lw[k] = tok
            rd[k] = {}

    def op(self, e, fn, reads=(), writes=(), extra=()):
        self._deps(e, reads, writes, extra)
        lst = self.ops.setdefault(e, [])
        rec = ["op", _eager(fn), False]
        lst.append(rec)
        self.q[e].append(rec)
        tok = Tok(e, len(lst))
        self._commit(tok, reads, writes)
        return tok

    def dma(self, e, sem, fn, reads=(), writes=(), extra=()):
        self._deps(e, reads, writes, extra)
        self.dma_sems.add(sem)
        lst = self.ops.setdefault(sem, [])
        rec = ["dma", _eager(fn), True, sem]
        lst.append(rec)
        self.q[e].append(rec)
        tok = Tok(sem, len(lst))
        self._commit(tok, reads, writes)
        return tok

    def collective(self, sem, fn, reads=(), writes=()):
        self._deps("pool", reads, writes, ())
        self.cc_sems.add(sem)
        rec = ["cc", _eager(fn), True, sem]
        self.ops.setdefault(sem, []).append(rec)
        self.q["pool"].append(rec)
        tok = Tok(sem, 1)
        self._commit(tok, reads, writes)
        self._wait("pool", tok)
        return tok

    def wait_all(self, e, toks):
        for t in toks:
            self._wait(e, t)

    def replay(self, nc, es):
        names = list(self.ENGS[:4]) + sorted(self.dma_sems) + sorted(self.cc_sems)
        sems = {n: es.enter_context(nc.semaphore("s_" + n)) for n in names}
        semval = {}
        for src, lst in self.ops.items():
            c = 0
            vals = []
            for rec in lst:
                if rec[2]:
                    c += 1
                vals.append(c)
            semval[src] = vals
        block = es.enter_context(nc.Block())
        engmap = {"pe": block.tensor, "act": block.scalar, "dve": block.vector, "pool": block.gpsimd, "sp": block.sync}

        def make(e):
            items = self.q[e]

            def body(eng):
                for it in items:
                    if it[0] == "wait":
                        src, idx = it[1], it[2]
                        mult = 16 if src in self.dma_sems else 1
                        eng.wait_ge(sems[src], semval[src][idx - 1] * mult)
                    elif it[0] == "op":
                        ins = it[1](eng)
                        if it[2]:
                            ins.then_inc(sems[e], 1)
                    elif it[0] == "cc":
                        it[1](eng).then_inc(sems[it[3]])
                    else:
                        it[1](eng).then_inc(sems[it[3]], 16)
            return body

        for e in self.ENGS:
            if self.q[e]:
                engmap[e](make(e))


def kchunk(name, c, tis):
    return [(name, c, t) for t in tis]


class MK:
    def __init__(self, nc, es, dbg=None, stop=None):
        self.nc = nc
        self.es = es
        self.b = Builder()
        self.dbg = dbg or []
        self.stop = stop
        self.psn = 0
        self.ps_mod = 8
        self.wsn = 0
        self._declare()

    def _dram_in(self, name, shape):
        return self.nc.dram_tensor(name, list(shape), F32, kind="ExternalInput").ap()

    def _sb(self, name, shape, dt):
        return self.es.enter_context(self.nc.sbuf_tensor(name, list(shape), dt))

    def _declare(self):
        nc = self.nc
        d = self._dram_in
        self.d_x = d("xT", [D, T])
        self.d_xp = d("xTp", [D, T])
        self.d_cst = d("cst", [128, NC])
        self.d_pc = d("pc", [128, NPC])
        self.d_smP = d("smP", [128, NSP])
        self.d_smB = d("smB", [L, 128, NSB])
        self.d_wfm = d("win_fm", [L, 128, 8, 1280])
        self.d_wkv = d("win_kv", [L, 128, 8, 384])
        self.d_wglrT = d("win_glrT", [L, 16, 1024])
        self.d_wg2 = d("wg2", [L, 16, 128])
        self.d_wrv = d("win_rv", [L, 128, 8, 512])
        self.d_wqk = d("win_qk", [L, 128, 8, 256])
        self.d_wg = d("win_g", [L, 128, 8, 4096])
        self.d_bproj = d("bproj", [L, 128, 8, 1024])
        self.d_wout = d("wout", [L, 128, 8, 1024])
        small = self.stop is not None and self.stop != "ffn1"
        self.d_f13 = d("ffn_w13", [11, 128, 8, 512])
        self.d_f2 = d("ffn_w2t", [2, 4, 128, 12, 256])
        self.d_m13 = d("moe_w13", [1, 1, 1, 1, 8] if small else [NE, 11, 128, 8, 512])
        self.d_m2 = d("moe_w2t", [1, 1, 1, 1, 1, 8] if small else [NE, 2, 4, 128, 12, 256])
        self.d_out = nc.dram_tensor("outT", [D, T], F32, kind="ExternalOutput").ap()
        self.d_dbg = {}
        for name in self.dbg:
            self.d_dbg[name] = nc.dram_tensor("dbg_" + name, [128, 8, T], F32, kind="ExternalOutput").ap()

        self.X = self._sb("X", [128, 8, T], F32)
        self.H = self._sb("H", [128, 8, T], BF16)
        self.YM = self._sb("YM", [128, 16, T], BF16)
        self.WS = self._sb("WS", [128, NSLOT, SLOT], BF16)
        self.cst = self._sb("cstS", [128, NC], F32)
        self.cstb = self._sb("cstB", [128, 392], BF16)
        self.pc = self._sb("pcS", [128, NPC], F32)
        self.smP = self._sb("smPS", [128, NSP], F32)
        self.RS = [self._sb("RS%d" % i, [128, 512], F32) for i in range(2)]
        self.Tm = [self._sb("Tm%d" % i, [128, 528], F32) for i in range(4)]
        self.S = self._sb("S", [128, 256], F32)
        self.dec = self._sb("dec", [128, 32], F32)
        self.pay = [self._sb("pay%d" % i, [128, XW], F32) for i in range(2)]
        self.xin = self._sb("xinS", [128, XW], F32)
        self.st4 = self._sb("st4", [128, 32], F32)
        self.st5 = self._sb("st5", [128, 32], F32)
        self.comb = self._sb("comb", [128, NT, 8], F32)
        self.PS = [self.es.enter_context(nc.psum_tensor("ps%d" % i, [128, 512], F32)) for i in range(8)]

    def ymflat(self):
        return self.YM[:].rearrange("p c t -> p (c t)")

    def ymkeys(self, off, n):
        ks = []
        e = (off // 128) * 128
        while e < off + n:
            ks.append(("Y", e // T, (e % T) // 128))
            e += 128
        return ks

    def ymv(self, off, n, dt=BF16):
        if dt == BF16:
            return self.ymflat()[:, off:off + n], self.ymkeys(off, n)
        v = self.ymflat()[:, off:off + 2 * n].bitcast(F32)
        return v, self.ymkeys(off, 2 * n)

    def ps(self):
        i = self.psn % self.ps_mod
        self.psn += 1
        return i

    def c(self, off, n):
        return self.cst[:, off:off + n]

    def wload(self, src_ap, shape_kc_n, extra_reads=()):
        s = self.wsn % NSLOT
        self.wsn += 1
        kc, n = shape_kc_n
        assert kc * n <= SLOT
        view = self.WS[:, s, 0:kc * n].rearrange("p (k n) -> p k n", k=kc)
        self.b.dma("pool", "w%d" % s, lambda g, o=view, i=src_ap: g.dma_start(out=o, in_=i),
                   reads=(), writes=[("W", s)])
        return view, s

    def mm(self, psi, out_ap, pairs, reads, first_start=True, last_stop=True):
        n = len(pairs)
        tok = None
        for i, (lt, rh) in enumerate(pairs):
            st = first_start and i == 0
            sp = last_stop and i == n - 1
            tok = self.b.op("pe", lambda pe, o=out_ap, a=lt, r=rh, st=st, sp=sp: pe.matmul(o, a, r, start=st, stop=sp),
                            reads=reads if i == 0 else (), writes=[("ps", psi)])
        self.b._commit(tok, reads, [])
        return tok

    def dve(self, fn, reads, writes):
        return self.b.op("dve", fn, reads, writes)

    def act(self, fn, reads, writes):
        return self.b.op("act", fn, reads, writes)

    def pool(self, fn, reads, writes):
        return self.b.op("pool", fn, reads, writes)

    def dump(self, name, src_fn, keys_fn):
        if name not in self.d_dbg:
            return
        for kc in range(8):
            self.b.dma("pool", "dbg", lambda g, o=self.d_dbg[name][:, kc, :], i=src_fn(kc): g.dma_start(out=o, in_=i),
                       reads=keys_fn(kc), writes=[("dbg", name, kc)])

    def kX(self, kc, b):
        return [("X", kc, 4 * b + i) for i in range(4)]

    def kH(self, kc, b):
        return [("H", kc, 4 * b + i) for i in range(4)]

    def kHall(self, b):
        return [("H", kc, 4 * b + i) for kc in range(8) for i in range(4)]

    def kHtile(self, i):
        return [("H", kc, i) for kc in range(8)]

    def kY(self, c, b):
        return [("Y", c, 4 * b + i) for i in range(4)]

    def init(self):
        b = self.b
        b.dma("sp", "m0", lambda q: q.dma_start(out=self.cst[:], in_=self.d_cst), writes=["cst"])
        b.dma("sp", "m1", lambda q: q.dma_start(out=self.pc[:], in_=self.d_pc), writes=["pc"])
        b.dma("sp", "m2", lambda q: q.dma_start(out=self.smP[:], in_=self.d_smP), writes=["smP"])
        self.ones_bf = self.cstb[:, 0:128]
        self.tri2_bf = self.cstb[:, 128:256]
        self.rev2_bf = self.cstb[:, 256:384]
        self.ch2_bf = self.cstb[:, 384:386]
        self.dve(lambda v: v.tensor_copy(self.cstb[:, 0:128], self.c(OC_ONES, 128)), ["cst"], ["cstb"])
        self.dve(lambda v: v.tensor_copy(self.cstb[:, 128:384], self.c(OC_TRI2, 256)), ["cst"], ["cstb"])
        self.dve(lambda v: v.tensor_copy(self.cstb[:, 384:386], self.c(OC_CH2, 2)), ["cst"], ["cstb"])

    def load_x(self, src):
        for kc in range(8):
            self.b.dma("sp", "xin%d" % kc, lambda q, kc=kc: q.dma_start(out=self.X[:, kc, :], in_=src[kc * 128:(kc + 1) * 128, :]),
                       writes=[("X", kc, i) for i in range(NT)])

    def rmsnorm(self, gain_off, out_fn, out_keys_fn, hook=None):
        sq_off = 8 * T
        for b in range(NB):
            blk = slice(b * 512, (b + 1) * 512)
            psi = self.ps()
            for half in range(2):
                sq, sqk = self.ymv(sq_off + half * T, T)
                sq3 = sq.rearrange("p (k t) -> p k t", k=4)
                xr = [k for kc in range(4 * half, 4 * half + 4) for k in self.kX(kc, b)]
                self.act(lambda a, o=sq3, i=self.X[:, 4 * half:4 * half + 4, blk]: a.activation(out=o, in_=i, func=AF.Square),
                         xr, sqk)
                self.mm(psi, self.PS[psi][:, :], [(self.ones_bf, sq3[:, k, :]) for k in range(4)],
                        reads=["cstb"] + sqk, first_start=(half == 0), last_stop=(half == 1))
            rt = self.Tm[0][:, 0:512]
            rs = self.RS[b % 2]
            self.act(lambda a, o=rt, i=self.PS[psi][:, :]: a.activation(out=o, in_=i, func=AF.Sqrt, bias=EPS, scale=1.0 / D),
                     [("ps", psi)], ["Tm0"])
            self.dve(lambda v, o=rs[:], i=rt: v.reciprocal(out=o, in_=i), ["Tm0"], [("RS", b % 2)])
            for kc in range(8):
                self.dve(lambda v, o=out_fn(kc, b), i=self.X[:, kc, blk], g=self.smP[:, gain_off + kc:gain_off + kc + 1], r=rs[:]:
                         v.scalar_tensor_tensor(out=o, in0=i, scalar=g, in1=r, op0=ALU.mult, op1=ALU.mult),
                         self.kX(kc, b) + [("RS", b % 2), "smP"], out_keys_fn(kc, b))
            if hook is not None:
                hook(b, rs)

    MB = 8 * T
    O_SMB = MB
    O_WST = MB + 3360
    O_POOLW = MB + 3872
    O_KEND = MB + 4128
    O_VTM = MB + 6176
    O_LTM = MB + 10272
    O_QH = MB + 12320
    O_QZ = MB + 14368
    O_KINV = MB + 15392

    def mixer(self, l, mode="main"):
        b = self.b
        self.invc_off = 8 if mode == "main" else 40
        state_only = mode == "pstate"
        Hh, Y = self.H, self.YM
        self.rmsnorm(OP_GMIX + 8 * l, lambda kc, bb: Hh[:, kc, bb * 512:(bb + 1) * 512], lambda kc, bb: self.kH(kc, bb))
        if self.stop == "norm%d" % l:
            return
        smB, smBk = self.ymv(self.O_SMB, NSB, F32)
        b.dma("sp", "misc", lambda q: q.dma_start(out=smB, in_=self.d_smB[l]), writes=smBk)
        sB = lambda off, n: smB[:, off:off + n]
        wst_bf, wstk = self.ymv(self.O_WST, 512)
        poolw_bf, poolwk = self.ymv(self.O_POOLW, 256)
        self.dve(lambda v: v.tensor_tensor(out=wst_bf.rearrange("p (h t) -> p h t", h=4), in0=sB(OB_WST, 512).rearrange("p (h t) -> p h t", h=4),
                                           in1=self.c(OC_TRISGU, 128).unsqueeze(1).to_broadcast([128, 4, 128]), op=ALU.mult),
                 smBk + ["cst"], wstk)
        self.dve(lambda v: v.tensor_copy(poolw_bf, sB(OB_POOLW, 256)), smBk, poolwk)
        kend, kendk = self.ymv(self.O_KEND, 2048)
        vtm, vtmk = self.ymv(self.O_VTM, 4096)
        ltm, ltmk = self.ymv(self.O_LTM, 2048)
        kend3 = kend.rearrange("p (i k) -> p i k", i=NT)
        vtm3 = vtm.rearrange("p (i k) -> p i k", i=NT)
        ltm3 = ltm.rearrange("p (i k) -> p i k", i=NT)
        kendk_i = lambda i: self.ymkeys(self.O_KEND + i * 128, 128)
        vtmk_i = lambda i: self.ymkeys(self.O_VTM + i * 256, 256)
        ltmk_i = lambda i: self.ymkeys(self.O_LTM + i * 128, 128)

        for g in ((0, 2) if state_only else (0, 1, 2)):
            ncol = 512 if g < 2 else 256
            W, s = self.wload(self.d_wfm[l][:, :, g * 512:g * 512 + ncol], (8, ncol))
            for bb in ((NB - 1,) if state_only else range(NB)):
                blk = slice(bb * 512, (bb + 1) * 512)
                pss = []
                for ct in range(ncol // 128):
                    psi = self.ps()
                    self.mm(psi, self.PS[psi][:, :], [(W[:, kc, ct * 128:(ct + 1) * 128], Hh[:, kc, blk]) for kc in range(8)],
                            reads=[("W", s)] + self.kHall(bb))
                    pss.append(psi)
                if g == 0:
                    for cc in range(2):
                        tm = self.Tm[cc][:, 0:512]
                        self.act(lambda a, o=tm, i=self.PS[pss[cc]][:, :]: a.copy(out=o, in_=i), [("ps", pss[cc])], ["Tm%d" % cc])
                        self.dve(lambda v, o=Y[:, 6 + cc, blk], a_=tm, p=self.PS[pss[2 + cc]][:, :]: v.tensor_tensor(out=o, in0=p, in1=a_, op=ALU.mult),
                                 ["Tm%d" % cc, ("ps", pss[2 + cc])], self.kY(6 + cc, bb))
                elif g == 1:
                    for cc in range(2):
                        self.act(lambda a, o=Y[:, cc, blk], i=self.PS[pss[cc]][:, :]: a.activation(out=o, in_=i, func=AF.Gelu_apprx_tanh),
                                 [("ps", pss[cc])], self.kY(cc, bb))
                        self.dve(lambda v, o=Y[:, 2 + cc, blk], i=self.PS[pss[2 + cc]][:, :]: v.tensor_copy(o, i),
                                 [("ps", pss[2 + cc])], self.kY(2 + cc, bb))
                else:
                    for cc in range(2):
                        self.dve(lambda v, o=Y[:, 4 + cc, blk], i=self.PS[pss[cc]][:, :]: v.tensor_copy(o, i),
                                 [("ps", pss[cc])], self.kY(4 + cc, bb))

        if self.stop == "a1_%d" % l:
            self.dump("Y%d" % l, lambda kc: Y[:, kc, :], lambda kc: [("Y", kc, i) for i in range(NT)])
            return
        tmpw, tmpwk = self.ymv(self.O_QH, 2048)
        wglrT_bf = tmpw[0:16, 0:1024]
        wg2_bf = tmpw[0:16, 1024:1152]
        b.dma("pool", "misc2", lambda g_: g_.dma_start(out=wglrT_bf, in_=self.d_wglrT[l]), writes=tmpwk)
        b.dma("pool", "misc2", lambda g_: g_.dma_start(out=wg2_bf, in_=self.d_wg2[l]), writes=tmpwk)
        s = self.wsn % NSLOT
        self.wsn += 1
        Wkv = self.WS[:, s, 0:4096].rearrange("p (k n) -> p k n", k=8)
        b.dma("pool", "w%d" % s, lambda g_: g_.dma_start(out=Wkv[:, :, 0:384], in_=self.d_wkv[l]), writes=[("W", s)])
        for half in range(2):
            psi = self.ps()
            for k4 in range(4):
                kc = half * 4 + k4
                self.mm(psi, self.PS[psi][:, k4 * 128:(k4 + 1) * 128], [(wglrT_bf[:, kc * 128:(kc + 1) * 128], wg2_bf)], reads=tmpwk)
            self.dve(lambda v, o=Wkv[:, half * 4:half * 4 + 4, 384:512], i=self.PS[psi][:, :].rearrange("p (k n) -> p k n", k=4): v.tensor_copy(o, i),
                     [("ps", psi)], [("W", s)])
        self.pool(lambda g_: g_.memset(self.S[:], 0.0), [], ["S"])
        for i in range(NT):
            tok = slice(i * 128, (i + 1) * 128)
            psA = self.ps()
            self.mm(psA, self.PS[psA][:, :], [(Hh[:, kc, tok], Wkv[:, kc, :]) for kc in range(8)], reads=[("W", s)] + self.kHtile(i))
            PA = self.PS[psA]
            t0 = self.Tm[2 * (i % 2)]
            t1 = self.Tm[2 * (i % 2) + 1]
            k0 = "Tm%d" % (2 * (i % 2))
            k1 = "Tm%d" % (2 * (i % 2) + 1)
            self.dve(lambda v, o=t0[:, 0:128], p=PA[:, 384:512]: v.tensor_tensor(out=o, in0=p, in1=sB(OB_BG, 128), op=ALU.add),
                     [("ps", psA)] + smBk, [k0])
            self.act(lambda a, o=t0[:, 128:256], i_=t0[:, 0:128]: a.activation(out=o, in_=i_, func=AF.Exp, scale=-1.0), [k0], [k0])
            self.act(lambda a, o=t0[:, 256:384], i_=t0[:, 128:256]: a.activation(out=o, in_=i_, func=AF.Ln, bias=1.0), [k0], [k0])
            self.dve(lambda v, o=ltm3[:, i, :], i_=t0[:, 256:384]: v.tensor_copy(o, i_), [k0], ltmk_i(i))
            psB = self.ps()
            PB = self.PS[psB]
            self.mm(psB, PB[:, 0:128], [(self.rev2_bf, ltm3[:, i, :])], reads=["cstb"] + ltmk_i(i))
            self.mm(psB, PB[:, 128:130], [(ltm3[:, i, :], self.ch2_bf)], reads=["cstb"] + ltmk_i(i))
            self.act(lambda a, o=t1[:, 0:128], i_=PB[:, 0:128]: a.activation(out=o, in_=i_, func=AF.Exp), [("ps", psB)], [k1])
            self.act(lambda a, o=self.dec[:, 2 * i:2 * i + 2], i_=PB[:, 128:130]: a.activation(out=o, in_=i_, func=AF.Exp), [("ps", psB)], [("dec", i)])
            self.dve(lambda v, o=kend3[:, i, :], p=PA[:, 0:128], e=t1[:, 0:128]: v.tensor_tensor(out=o, in0=p, in1=e, op=ALU.mult),
                     [("ps", psA), k1], kendk_i(i))
            self.dve(lambda v, o=vtm3[:, i, :], p=PA[:, 128:384]: v.tensor_copy(o, p), [("ps", psA)], vtmk_i(i))
            psK = self.ps()
            for j in range(2):
                rows = slice(64 * j, 64 * j + 64)
                self.mm(psK, self.PS[psK][:, j * 256:(j + 1) * 256], [(kend3[rows, i, :], vtm3[rows, i, :])], reads=kendk_i(i) + vtmk_i(i))
                self.dve(lambda v, p=self.PS[psK][:, j * 256:(j + 1) * 256], d_=self.dec[:, 2 * i + j:2 * i + j + 1]:
                         v.scalar_tensor_tensor(out=self.S[:], in0=self.S[:], scalar=d_, in1=p, op0=ALU.mult, op1=ALU.add),
                         [("ps", psK), ("dec", i), "S"], ["S"])
        if self.stop == "a2_%d" % l and mode == "main":
            return
        if mode != "main":
            pay = self.pay[l]
            pk = ("pay", l)
            t2 = self.Tm[2]
            self.dve(lambda v: v.tensor_tensor(out=t2[:, 0:256], in0=self.S[:], in1=self.c(OC_BMASK, 256), op=ALU.mult), ["S", "cst"], ["Tm2"])
            self.dve(lambda v: v.tensor_reduce(out=pay[:, 0:64], in_=t2[:, 0:256].rearrange("p (h v) -> p v h", h=4), axis=AX.X, op=ALU.add),
                     ["Tm2"], [pk])
            self.dve(lambda v: v.tensor_copy(pay[:, 64:96].rearrange("p (c t) -> p c t", c=2), Y[:, 4:6, T - 16:T]),
                     [("Y", 4, 15), ("Y", 5, 15)], [pk])
            self.dve(lambda v: v.tensor_copy(pay[:, 96:100].rearrange("p (c t) -> p c t", c=2), Y[:, 6:8, T - 2:T]),
                     [("Y", 6, 15), ("Y", 7, 15)], [pk])
            if state_only:
                return
            self.pool(lambda g_: g_.memset(self.xin[:], 0.0), [], ["xinS"])
        else:
            self.dve(lambda v: v.tensor_scalar(out=self.xin[:], in0=self.pay[l][:], scalar1=self.pc[:, 0:1], scalar2=None, op0=ALU.mult),
                     [("pay", l), "pc"], ["xinS"])
        self.dve(lambda v: v.tensor_tensor(out=self.S[:].rearrange("p (h v) -> p h v", h=4), in0=self.c(OC_BMASK, 256).rearrange("p (h v) -> p h v", h=4),
                                           in1=self.xin[:, 0:64].unsqueeze(1).to_broadcast([128, 4, 64]), op=ALU.mult),
                 ["xinS", "cst", "S"], ["S"])
        if self.stop == "x_%d" % l:
            return
        self._mixer_b(l, smB, smBk, poolw_bf, poolwk)
        if self.stop == "b_%d" % l:
            self.dump("Y%d" % l, lambda kc: Y[:, kc, :], lambda kc: [("Y", kc, i) for i in range(NT)])
            return
        self._mixer_c(l, smB, smBk, wst_bf, wstk, kend3, kendk_i, vtm3, vtmk_i, ltm3, ltmk_i)
        self.dump("Y%d" % l, lambda kc: Y[:, kc, :], lambda kc: [("Y", kc, i) for i in range(NT)])
        if self.stop == "branch%d" % l:
            return
        self._mixer_gate(l)

    def _mixer_b(self, l, smB, smBk, poolw_bf, poolwk):
        Y = self.YM
        sB = lambda off, n: smB[:, off:off + n]
        for bb in range(NB):
            t0 = bb * 512
            blk = slice(t0, t0 + 512)
            for cc in range(2):
                ta, tb = self.Tm[2 * cc], self.Tm[2 * cc + 1]
                ka, kb_ = "Tm%d" % (2 * cc), "Tm%d" % (2 * cc + 1)
                if bb == 0:
                    self.act(lambda a, o=ta[:, 2:514], i=Y[:, 6 + cc, 0:512]: a.copy(out=o, in_=i), self.kY(6 + cc, 0), [ka])
                    self.act(lambda a, o=ta[:, 0:2], i=self.xin[:, 96 + 2 * cc:98 + 2 * cc]: a.copy(out=o, in_=i), ["xinS"], [ka])
                else:
                    self.act(lambda a, o=ta[:, 0:514], i=Y[:, 6 + cc, t0 - 2:t0 + 512]: a.copy(out=o, in_=i),
                             self.kY(6 + cc, bb) + [("Y", 6 + cc, 4 * bb - 1)], [ka])
                cw = lambda k: sB(OB_CONVW + 2 * k + cc, 1)
                self.act(lambda a, o=tb[:, 0:512], i=ta[:, 2:514]: a.activation(out=o, in_=i, func=AF.Identity, bias=sB(OB_CONVB + cc, 1), scale=cw(2)),
                         [ka] + smBk, [kb_])
                self.dve(lambda v, o=tb[:, 0:512], i=ta[:, 1:513]: v.scalar_tensor_tensor(out=o, in0=i, scalar=cw(1), in1=o, op0=ALU.mult, op1=ALU.add),
                         [ka, kb_] + smBk, [kb_])
                self.dve(lambda v, o=tb[:, 0:512], i=ta[:, 0:512]: v.scalar_tensor_tensor(out=o, in0=i, scalar=cw(0), in1=o, op0=ALU.mult, op1=ALU.add),
                         [ka, kb_] + smBk, [kb_])
                self.dve(lambda v, o=Y[:, 2 + cc, blk], i=tb[:, 0:512]: v.tensor_tensor(out=o, in0=i, in1=o, op=ALU.mult),
                         [kb_] + self.kY(2 + cc, bb), self.kY(2 + cc, bb))
        pb_all = self.RS[1][:].bitcast(BF16)
        for bb in range(NB - 1, -1, -1):
            t0 = bb * 512
            blk = slice(t0, t0 + 512)
            for cc in range(2):
                Z, s2, s4, s8 = self.Tm[0], self.Tm[1], self.Tm[2], self.Tm[3]
                if bb == 0:
                    self.act(lambda a, o=Z[:, 16:528], i=Y[:, 4 + cc, 0:512]: a.copy(out=o, in_=i), self.kY(4 + cc, 0), ["Tm0"])
                    self.act(lambda a, o=Z[:, 0:16], i=self.xin[:, 64 + 16 * cc:80 + 16 * cc]: a.copy(out=o, in_=i), ["xinS"], ["Tm0"])
                else:
                    self.act(lambda a, o=Z[:, 0:528], i=Y[:, 4 + cc, t0 - 16:t0 + 512]: a.copy(out=o, in_=i),
                             self.kY(4 + cc, bb) + [("Y", 4 + cc, 4 * bb - 1)], ["Tm0"])
                self.dve(lambda v: v.tensor_tensor(out=s2[:, 1:528], in0=Z[:, 1:528], in1=Z[:, 0:527], op=ALU.add), ["Tm0"], ["Tm1"])
                self.dve(lambda v: v.tensor_tensor(out=s4[:, 3:528], in0=s2[:, 3:528], in1=s2[:, 1:526], op=ALU.add), ["Tm1"], ["Tm2"])
                if cc == 0:
                    lo, hi, klo, khi = s2, s4, "Tm1", "Tm2"
                else:
                    self.dve(lambda v: v.tensor_tensor(out=s8[:, 7:528], in0=s4[:, 7:528], in1=s4[:, 3:524], op=ALU.add), ["Tm2"], ["Tm3"])
                    self.dve(lambda v: v.tensor_tensor(out=s2[:, 15:528], in0=s8[:, 15:528], in1=s8[:, 7:520], op=ALU.add), ["Tm3"], ["Tm1"])
                    lo, hi, klo, khi = s8, s2, "Tm3", "Tm1"
                pb = pb_all[:, cc * 512:(cc + 1) * 512]
                pbk = ("PB", cc)
                for (lev, klev, rows) in ((lo, klo, slice(0, 64)), (hi, khi, slice(64, 128))):
                    self.dve(lambda v, lev=lev, rows=rows: v.scalar_tensor_tensor(out=pb[rows, :], in0=lev[rows, 16:528], scalar=sB(OB_INVW + cc, 1)[rows, :],
                                                                                 in1=Z[rows, 16:528], op0=ALU.mult, op1=ALU.subtract),
                             [klev, "Tm0", ("RS", 1)] + smBk, [pbk, ("RS", 1)])
                    if bb == 0:
                        tq = self.st4[:, 0:16]
                        self.dve(lambda v, lev=lev, rows=rows: v.tensor_tensor(out=tq[rows, :], in0=lev[rows, 16:32], in1=self.pc[rows, self.invc_off + 16 * cc:self.invc_off + 16 + 16 * cc], op=ALU.mult),
                                 [klev, "pc"], ["st4"])
                        self.dve(lambda v, rows=rows: v.tensor_tensor(out=pb[rows, 0:16], in0=tq[rows, :], in1=Z[rows, 16:32], op=ALU.subtract),
                                 ["st4", "Tm0", pbk], [pbk])
                psi = self.ps()
                self.mm(psi, self.PS[psi][:, :], [(poolw_bf[:, cc * 128:(cc + 1) * 128], pb)], reads=poolwk + [pbk])
                self.act(lambda a, o=Y[:, 4 + cc, blk], i=self.PS[psi][:, :]: a.activation(out=o, in_=i, func=AF.Identity, scale=sB(OB_PSC + cc, 1)),
                         [("ps", psi)] + smBk, self.kY(4 + cc, bb))

    def _mixer_c(self, l, smB, smBk, wst_bf, wstk, kend3, kendk_i, vtm3, vtmk_i, ltm3, ltmk_i):
        b = self.b
        Y, Hh = self.YM, self.H
        sB = lambda off, n: smB[:, off:off + n]
        Wrv, s_rv = self.wload(self.d_wrv[l], (8, 512))
        Wqk, s_qk = self.wload(self.d_wqk[l], (8, 256))
        qh, qhk = self.ymv(self.O_QH, 2048)
        qz, qzk = self.ymv(self.O_QZ, 1024)
        kinv, kinvk = self.ymv(self.O_KINV, 512)
        qh3 = qh.rearrange("p (h t) -> p h t", h=4)
        qz4 = qz.rearrange("p (i j t) -> p i j t", i=4, j=2)
        scm_all = self.RS[0][:].bitcast(BF16)
        wst3 = wst_bf.rearrange("p (h t) -> p h t", h=4)
        s_free = self.wsn % NSLOT
        ext = self.WS[:, s_free, :].bitcast(F32)
        KW = ("W", s_free)
        self.pool(lambda g_: g_.memset(qz, 0.0), [], qzk)
        self.ps_mod = 6
        for bb in range(NB):
            blk = slice(bb * 512, (bb + 1) * 512)
            psQ, psK, psC = self.ps(), self.ps(), self.ps()
            self.mm(psQ, self.PS[psQ][:, :], [(Wqk[:, kc, 0:128], Hh[:, kc, blk]) for kc in range(8)], reads=[("W", s_qk)] + self.kHall(bb))
            self.mm(psK, self.PS[psK][:, :], [(Wqk[:, kc, 128:256], Hh[:, kc, blk]) for kc in range(8)], reads=[("W", s_qk)] + self.kHall(bb))
            for ti in range(4):
                i = 4 * bb + ti
                self.mm(psC, self.PS[psC][:, ti * 128:(ti + 1) * 128], [(ltm3[:, i, :], self.tri2_bf)], reads=["cstb"] + ltmk_i(i))
            ec, en = self.Tm[0][:, 0:512], self.Tm[1][:, 0:512]
            self.act(lambda a, i_=self.PS[psC][:, :]: a.activation(out=ec, in_=i_, func=AF.Exp), [("ps", psC)], ["Tm0"])
            self.act(lambda a, i_=self.PS[psC][:, :]: a.activation(out=en, in_=i_, func=AF.Exp, scale=-1.0), [("ps", psC)], ["Tm1"])
            for h in range(4):
                self.dve(lambda v, h=h, p=self.PS[psQ][:, :]: v.scalar_tensor_tensor(out=qh3[:, h, :], in0=p, scalar=self.c(OC_HM + h, 1), in1=ec,
                                                                                  op0=ALU.mult, op1=ALU.mult), [("ps", psQ), "Tm0", "cst"], qhk)
            self.dve(lambda v, p=self.PS[psK][:, :]: v.tensor_tensor(out=kinv, in0=p, in1=en, op=ALU.mult), [("ps", psK), "Tm1"], kinvk)
            for j in range(2):
                self.dve(lambda v, j=j, p=self.PS[psQ][:, :]: v.scalar_tensor_tensor(
                    out=qz4[:, :, j, 64 * j:64 * j + 64], in0=p.rearrange("p (i j t) -> p i j t", i=4, j=2)[:, :, j, :], scalar=float(32 ** -0.5),
                    in1=ec.rearrange("p (i j t) -> p i j t", i=4, j=2)[:, :, j, :], op0=ALU.mult, op1=ALU.mult), [("ps", psQ), "Tm0"], qzk)
            def make_tile(i, ti):
                tok = slice(i * 128, (i + 1) * 128)
                tl = slice(ti * 128, (ti + 1) * 128)
                box = {}

                def sgu():
                    psR = 6 + (i % 2)
                    PR = self.PS[psR]
                    self.mm(psR, PR[:, :], [(Hh[:, kc, tok], Wrv[:, kc, :]) for kc in range(8)], reads=[("W", s_rv)] + self.kHtile(i))
                    yield
                    par = i % 2
                    if par == 0:
                        t2, t3a, sb_all, wide, st = self.Tm[2][:, 0:512], self.Tm[3][:, 0:256], self.Tm[3][:, 256:512].bitcast(BF16), self.RS[1][:, :], self.st4
                        XK, XW_ = [], []
                    else:
                        t2, t3a, sb_all, wide, st = ext[:, 0:512], ext[:, 512:768], ext[:, 768:1024].bitcast(BF16), ext[:, 1024:1536], self.st5
                        XK, XW_ = [KW], [KW]
                    kT2, kT2b, kT3, kSt, kStg, kWA, kWB, kRS1 = ("Tm2%d" % par, "Tm2b%d" % par, "Tm3%d" % par, "st4%d" % par, "st4g%d" % par, "wA%d" % par, "wB%d" % par, ("RS", 1) if par == 0 else KW)
                    if par == 0:
                        kT2, kT2b, kT3, kSt = "Tm2", "Tm2b", "Tm3", "st4"
                    vg, sq = t2[:, 0:256], t2[:, 256:512]
                    self.act(lambda a: a.activation(out=vg, in_=PR[:, 256:512], func=AF.Gelu_apprx_tanh), [("ps", psR)], [kT2] + XW_)
                    yield
                    self.act(lambda a: a.activation(out=sq, in_=vg, func=AF.Square), [kT2] + XK, [kT2b])
                    yield
                    self.dve(lambda v: v.tensor_reduce(out=st[:, 0:4], in_=sq.rearrange("p (h d) -> p h d", h=4), axis=AX.X, op=ALU.add), [kT2b] + XK, [kSt])
                    yield
                    self.act(lambda a: a.activation(out=st[:, 4:8], in_=st[:, 0:4], func=AF.Sqrt, bias=EPS, scale=1.0 / 64), [kSt], [kSt])
                    yield
                    self.dve(lambda v: v.reciprocal(out=st[:, 8:12], in_=st[:, 4:8]), [kSt], [kSt])
                    yield
                    self.dve(lambda v: v.tensor_tensor(out=vg.rearrange("p (h d) -> p h d", h=4), in0=vg.rearrange("p (h d) -> p h d", h=4),
                                                       in1=st[:, 8:12].unsqueeze(2).to_broadcast([128, 4, 64]), op=ALU.mult), [kT2, kSt] + XK, [kT2])
                    yield
                    vnb = sq.bitcast(BF16)[:, 0:256]
                    self.dve(lambda v: v.tensor_tensor(out=vnb, in0=vg, in1=sB(OB_SGUN, 256), op=ALU.mult), [kT2, kT2b] + smBk + XK, [kT2b])
                    yield
                    psM = self.ps()
                    PM = self.PS[psM]
                    for h in range(4):
                        cc, hh = h // 2, h % 2
                        self.mm(psM, PM[64 * hh:64 * hh + 64, cc * 128:(cc + 1) * 128], [(vnb[:, 64 * h:64 * h + 64], wst3[:, h, :])], reads=[kT2b] + wstk + XK)
                    self.dve(lambda v: v.tensor_tensor(out=t3a, in0=PM[:, 0:256], in1=sB(OB_SGUB, 256), op=ALU.add), [("ps", psM)] + smBk + XK, [kT3])
                    yield
                    self.dve(lambda v, tok=tok: v.tensor_tensor(out=Y[:, 0:2, tok], in0=t3a.rearrange("p (c t) -> p c t", c=2), in1=Y[:, 0:2, tok], op=ALU.mult),
                             [kT3, ("Y", 0, i), ("Y", 1, i)] + XK, [("Y", 0, i), ("Y", 1, i)])
                    yield
                    box.update(psR=psR, PR=PR, par=par, sb_all=sb_all, wide=wide, st=st, XK=XK, kStg=kStg, kWA=kWA, kWB=kWB)

                def gla():
                    psR, PR, par, sb_all, wide, st, XK, kStg, kWA, kWB = (box[k] for k in ("psR", "PR", "par", "sb_all", "wide", "st", "XK", "kStg", "kWA", "kWB"))
                    psV = self.ps()
                    for j in range(2):
                        rows = slice(64 * j, 64 * j + 64)
                        sbj = sb_all[:, j * 256:(j + 1) * 256]
                        self.dve(lambda v, sbj=sbj: v.tensor_tensor(out=sbj, in0=self.S[:], in1=self.c(OC_BMASK, 256), op=ALU.mult), ["S", "cst"] + XK, [("Sb", j, par)])
                        self.mm(psV, self.PS[psV][:, j * 256:(j + 1) * 256], [(kend3[rows, i, :], vtm3[rows, i, :])], reads=kendk_i(i) + vtmk_i(i))
                        self.dve(lambda v, p=self.PS[psV][:, j * 256:(j + 1) * 256], d_=self.dec[:, 2 * i + j:2 * i + j + 1]:
                                 v.scalar_tensor_tensor(out=self.S[:], in0=self.S[:], scalar=d_, in1=p, op0=ALU.mult, op1=ALU.add),
                                 [("ps", psV), ("dec", i), "S"], ["S"])
                    psS = self.ps()
                    PSs = self.PS[psS]
                    for h in range(4):
                        self.mm(psS, PSs[:, h * 128:(h + 1) * 128], [(kinv[:, tl], qh3[:, h, tl])], reads=kinvk + qhk)
                    scm = scm_all[:, (i % 2) * 512:(i % 2) * 512 + 512]
                    scmk = ("scm", i % 2)
                    self.dve(lambda v, scm=scm: v.tensor_tensor(out=scm.rearrange("p (h t) -> p h t", h=4), in0=PSs[:, :].rearrange("p (h t) -> p h t", h=4),
                                                               in1=self.c(OC_CAUS2, 128).unsqueeze(1).to_broadcast([128, 4, 128]), op=ALU.mult),
                             [("ps", psS), "cst", ("RS", 0)], [scmk, ("RS", 0)])
                    yield
                    psO = self.ps()
                    PO = self.PS[psO]
                    pairs_reads = [("Sb", 0, par), ("Sb", 1, par), scmk] + qzk + vtmk_i(i) + XK
                    self.b.op("pe", lambda pe: pe.matmul(PO[:, 0:256], qz4[:, ti, 0, :], sb_all[:, 0:256], start=True, stop=False),
                              reads=pairs_reads, writes=[("ps", psO)])
                    self.b.op("pe", lambda pe: pe.matmul(PO[:, 0:256], qz4[:, ti, 1, :], sb_all[:, 256:512], start=False, stop=False),
                              reads=(), writes=[("ps", psO)])
                    tk = None
                    for h in range(4):
                        tk = self.b.op("pe", lambda pe, h=h, scm=scm: pe.matmul(PO[:, 64 * h:64 * h + 64], scm[:, h * 128:(h + 1) * 128], vtm3[:, i, 64 * h:64 * h + 64],
                                                                              start=False, stop=(h == 3)), reads=(), writes=[("ps", psO)])
                    self.b._commit(tk, pairs_reads, [])
                    yield
                    osq, sr = wide[:, 0:256], wide[:, 256:512]
                    self.act(lambda a: a.activation(out=osq, in_=PO[:, 0:256], func=AF.Square), [("ps", psO)] + XK, [kWA] + ([("RS", 1)] if par == 0 else []))
                    yield
                    self.dve(lambda v: v.tensor_reduce(out=st[:, 16:20], in_=osq.rearrange("p (h d) -> p h d", h=4), axis=AX.X, op=ALU.add), [kWA] + XK, [kStg])
                    yield
                    self.act(lambda a: a.activation(out=st[:, 20:24], in_=st[:, 16:20], func=AF.Sqrt, bias=EPS, scale=1.0 / 64), [kStg], [kStg])
                    yield
                    self.dve(lambda v: v.reciprocal(out=st[:, 24:28], in_=st[:, 20:24]), [kStg], [kStg])
                    yield
                    self.dve(lambda v: v.tensor_tensor(out=osq.rearrange("p (h d) -> p h d", h=4), in0=PO[:, 0:256].rearrange("p (h d) -> p h d", h=4),
                                                       in1=st[:, 24:28].unsqueeze(2).to_broadcast([128, 4, 64]), op=ALU.mult), [("ps", psO), kStg, kWA] + XK, [kWA])
                    yield
                    self.dve(lambda v: v.tensor_tensor(out=osq, in0=osq, in1=sB(OB_GLAN, 256), op=ALU.mult), [kWA] + smBk + XK, [kWA])
                    yield
                    self.act(lambda a: a.activation(out=sr, in_=PR[:, 0:256], func=AF.Silu), [("ps", psR)] + XK, [kWB] + ([("RS", 1)] if par == 0 else []))
                    yield
                    self.dve(lambda v: v.tensor_tensor(out=osq, in0=osq, in1=sr, op=ALU.mult), [kWA, kWB] + XK, [kWA])
                    yield
                    psT = self.ps()
                    PT = self.PS[psT]
                    for cc in range(2):
                        self.b.op("pe", lambda pe, cc=cc: pe.transpose(PT[:, cc * 128:(cc + 1) * 128], osq[:, cc * 128:(cc + 1) * 128], self.c(OC_IDENT, 128)),
                                  reads=[kWA, "cst"] + XK, writes=[("ps", psT)])
                    self.act(lambda a, tok=tok: a.copy(out=Y[:, 6:8, tok], in_=PT[:, 0:256].rearrange("p (c t) -> p c t", c=2)),
                             [("ps", psT)], [("Y", 6, i), ("Y", 7, i)])


                    yield
                return sgu(), gla()

            def run_pair(g1, g2):
                gens = [g for g in (g1, g2) if g is not None]
                while gens:
                    for g in list(gens):
                        try:
                            next(g)
                        except StopIteration:
                            gens.remove(g)

            pend = None
            for ti in range(4):
                A, B = make_tile(4 * bb + ti, ti)
                run_pair(A, pend)
                pend = B
            run_pair(pend, None)
        self.ps_mod = 8

    def _mixer_gate(self, l):
        Y, Hh, X = self.YM, self.H, self.X
        for j in range(8):
            Wg, sg = self.wload(self.d_wg[l, j] if False else self.d_wg[l][:, :, j * 512:(j + 1) * 512], (8, 512))
            Pj, sp_ = self.wload(self.d_bproj[l][:, :, j * 128:(j + 1) * 128], (8, 128))
            for bb in range(NB):
                blk = slice(bb * 512, (bb + 1) * 512)
                acc, tmp = self.Tm[2][:, 0:512], self.Tm[3][:, 0:512]
                for i in range(4):
                    psG, psP = self.ps(), self.ps()
                    self.mm(psG, self.PS[psG][:, :], [(Wg[:, kc, i * 128:(i + 1) * 128], Hh[:, kc, blk]) for kc in range(8)],
                            reads=[("W", sg)] + self.kHall(bb))
                    self.mm(psP, self.PS[psP][:, :], [(Pj[:, 2 * i + c, :], Y[:, 2 * i + c, blk]) for c in range(2)],
                            reads=[("W", sp_)] + self.kY(2 * i, bb) + self.kY(2 * i + 1, bb))
                    gt = self.Tm[i % 2][:, 0:512]
                    gk = "Tm%d" % (i % 2)
                    col = OP_BGATE + 32 * l + i * 8 + j
                    self.act(lambda a, gt=gt, p=self.PS[psG][:, :], col=col: a.activation(out=gt, in_=p, func=AF.Sigmoid, bias=self.smP[:, col:col + 1]),
                             [("ps", psG), "smP"], [gk])
                    pp = self.PS[psP][:, :]
                    if i == 0:
                        self.dve(lambda v, gt=gt, pp=pp: v.tensor_tensor(out=acc, in0=pp, in1=gt, op=ALU.mult), [("ps", psP), gk], ["Tm2"])
                    else:
                        self.dve(lambda v, gt=gt, pp=pp: v.tensor_tensor(out=tmp, in0=pp, in1=gt, op=ALU.mult), [("ps", psP), gk], ["Tm3"])
                        if i < 3:
                            self.dve(lambda v: v.tensor_tensor(out=acc, in0=acc, in1=tmp, op=ALU.add), ["Tm2", "Tm3"], ["Tm2"])
                        else:
                            self.dve(lambda v, o=Y[:, 8 + j, blk]: v.tensor_tensor(out=o, in0=acc, in1=tmp, op=ALU.add), ["Tm2", "Tm3"], self.kY(8 + j, bb))
        self.dump("M%d" % l, lambda kc: Y[:, 8 + kc, :], lambda kc: [("Y", 8 + kc, i) for i in range(NT)])
        if self.stop == "gate%d" % l:
            return
        for dh in range(2):
            Wo, so = self.wload(self.d_wout[l][:, :, dh * 512:(dh + 1) * 512], (8, 512))
            for dd in range(4):
                dt = dh * 4 + dd
                for bb in range(NB):
                    blk = slice(bb * 512, (bb + 1) * 512)
                    psi = self.ps()
                    self.mm(psi, self.PS[psi][:, :], [(Wo[:, j, dd * 128:(dd + 1) * 128], Y[:, 8 + j, blk]) for j in range(8)],
                            reads=[("W", so)] + [k for j in range(8) for k in self.kY(8 + j, bb)])
                    self.dve(lambda v, o=X[:, dt, blk], p=self.PS[psi][:, :]: v.tensor_tensor(out=o, in0=p, in1=o, op=ALU.add),
                             [("ps", psi)] + self.kX(dt, bb), self.kX(dt, bb))
        self.dump("XM%d" % l, lambda kc: X[:, kc, :], lambda kc: [("X", kc, i) for i in range(NT)])

    FSPLIT = (12, 10)

    def ffn(self, d_w13, d_w2, cb=None, cbk=None):
        Y, Hh, X = self.YM, self.H, self.X
        f0 = 0
        for fh in range(2):
            nf = self.FSPLIT[fh]
            for g in range(nf // 2):
                W, s = self.wload(d_w13[f0 // 2 + g], (8, 512))
                for f2 in range(2):
                    fl = 2 * g + f2
                    for bb in range(NB):
                        blk = slice(bb * 512, (bb + 1) * 512)
                        ps1, ps3 = self.ps(), self.ps()
                        self.mm(ps1, self.PS[ps1][:, :], [(W[:, kc, f2 * 128:(f2 + 1) * 128], Hh[:, kc, blk]) for kc in range(8)],
                                reads=[("W", s)] + self.kHall(bb))
                        self.mm(ps3, self.PS[ps3][:, :], [(W[:, kc, 256 + f2 * 128:256 + (f2 + 1) * 128], Hh[:, kc, blk]) for kc in range(8)],
                                reads=[("W", s)] + self.kHall(bb))
                        n = (fl * NB + bb) % 2
                        sl = self.Tm[n][:, 0:512]
                        self.act(lambda a, sl=sl, p=self.PS[ps1][:, :]: a.activation(out=sl, in_=p, func=AF.Silu), [("ps", ps1)], ["Tm%d" % n])
                        if cb is None:
                            self.dve(lambda v, o=Y[:, fl, blk], sl=sl, p=self.PS[ps3][:, :]: v.tensor_tensor(out=o, in0=p, in1=sl, op=ALU.mult),
                                     [("ps", ps3), "Tm%d" % n], self.kY(fl, bb))
                        else:
                            t2 = self.Tm[2 + n][:, 0:512]
                            self.dve(lambda v, t2=t2, sl=sl, p=self.PS[ps3][:, :]: v.tensor_tensor(out=t2, in0=p, in1=sl, op=ALU.mult),
                                     [("ps", ps3), "Tm%d" % n], ["Tm%d" % (2 + n)])
                            self.dve(lambda v, o=Y[:, fl, blk], t2=t2, c_=cb[:, blk]: v.tensor_tensor(out=o, in0=t2, in1=c_, op=ALU.mult),
                                     ["Tm%d" % (2 + n)] + cbk(bb), self.kY(fl, bb))
            for dq in range(4):
                W2, s2 = self.wload(d_w2[fh, dq], (12, 256))
                for dd in range(2):
                    dt = dq * 2 + dd
                    for bb in range(NB):
                        blk = slice(bb * 512, (bb + 1) * 512)
                        psi = self.ps()
                        self.mm(psi, self.PS[psi][:, :], [(W2[:, f, dd * 128:(dd + 1) * 128], Y[:, f, blk]) for f in range(nf)],
                                reads=[("W", s2)] + [k for f in range(nf) for k in self.kY(f, bb)])
                        self.dve(lambda v, o=X[:, dt, blk], p=self.PS[psi][:, :]: v.tensor_tensor(out=o, in0=p, in1=o, op=ALU.add),
                                 [("ps", psi)] + self.kX(dt, bb), self.kX(dt, bb))
            f0 += nf

    def router_hook(self, gR):
        X = self.X

        def hook(bb, rs):
            for ti in range(4):
                i = 4 * bb + ti
                tok = slice(i * 128, (i + 1) * 128)
                psi = self.ps()
                P = self.PS[psi]
                self.mm(psi, P[:, 0:8], [(X[:, kc, tok], gR[:, kc * 8:(kc + 1) * 8]) for kc in range(8)],
                        reads=[("X", kc, i) for kc in range(8)] + ["gR"])
                self.b.op("pe", lambda pe, ti=ti: pe.transpose(P[:, 128:256], rs[:, ti * 128:(ti + 1) * 128], self.c(OC_IDENT, 128)),
                          reads=[("RS", bb % 2), "cst"], writes=[("ps", psi)])
                r = self.Tm[3]
                lg, eq1, l2, eq2 = r[:, 0:8], r[:, 8:16], r[:, 16:24], r[:, 24:32]
                m1, m2, dm, e2, w1, w2, rsd = (r[:, 32 + k:33 + k] for k in range(7))
                K = ["Tm3"]
                self.dve(lambda v: v.tensor_copy(lg, P[:, 0:8]), [("ps", psi)], K)
                self.dve(lambda v: v.tensor_copy(rsd, P[:, 128:129]), [("ps", psi)], K)
                self.dve(lambda v: v.reduce_max(out=m1, in_=lg, axis=AX.X), K, K)
                self.dve(lambda v: v.tensor_scalar(out=eq1, in0=lg, scalar1=m1, scalar2=None, op0=ALU.is_equal), K, K)
                self.dve(lambda v: v.scalar_tensor_tensor(out=l2, in0=eq1, scalar=-1e30, in1=lg, op0=ALU.mult, op1=ALU.add), K, K)
                self.dve(lambda v: v.reduce_max(out=m2, in_=l2, axis=AX.X), K, K)
                self.dve(lambda v: v.tensor_scalar(out=eq2, in0=l2, scalar1=m2, scalar2=None, op0=ALU.is_equal), K, K)
                self.dve(lambda v: v.tensor_tensor(out=dm, in0=m2, in1=m1, op=ALU.subtract), K, K)
                self.dve(lambda v: v.tensor_tensor(out=dm, in0=dm, in1=rsd, op=ALU.mult), K, K)
                self.act(lambda a: a.activation(out=e2, in_=dm, func=AF.Exp), K, K)
                self.dve(lambda v: v.tensor_scalar(out=w1, in0=e2, scalar1=1.0, scalar2=None, op0=ALU.add), K, K)
                self.dve(lambda v: v.reciprocal(out=w1, in_=w1), K, K)
                self.dve(lambda v: v.tensor_tensor(out=w2, in0=e2, in1=w1, op=ALU.mult), K, K)
                self.dve(lambda v, i=i: v.tensor_scalar(out=self.comb[:, i, :], in0=eq1, scalar1=w1, scalar2=None, op0=ALU.mult), K, [("comb", i)])
                self.dve(lambda v, i=i: v.scalar_tensor_tensor(out=self.comb[:, i, :], in0=eq2, scalar=w2, in1=self.comb[:, i, :], op0=ALU.mult, op1=ALU.add),
                         K + [("comb", i)], [("comb", i)])
        return hook

    def moe(self, l):
        Y = self.YM
        gR = self.Tm[2][:, 0:64]
        for kc in range(8):
            self.dve(lambda v, kc=kc: v.tensor_scalar(out=gR[:, kc * 8:(kc + 1) * 8], in0=self.smP[:, OP_ROUTER + kc * 8:OP_ROUTER + (kc + 1) * 8],
                                                      scalar1=self.smP[:, OP_GFFN + 8 * l + kc:OP_GFFN + 8 * l + kc + 1], scalar2=None, op0=ALU.mult),
                     ["smP"], ["gR", "Tm2"])
        self.rmsnorm(OP_GFFN + 8 * l, lambda kc, bb: self.H[:, kc, bb * 512:(bb + 1) * 512], lambda kc, bb: self.kH(kc, bb), hook=self.router_hook(gR))
        for e in range(NE):
            cbc = 12 + (e % 2)
            cb = Y[:, cbc, :]
            for bb in range(NB):
                psi = self.ps()
                for ti in range(4):
                    i = 4 * bb + ti
                    dg = Y[:, 14, (i % 4) * 128:(i % 4) * 128 + 128]
                    dgk = [("Y", 14, i % 4)]
                    self.dve(lambda v, dg=dg, i=i, e=e: v.tensor_scalar(out=dg, in0=self.c(OC_IDENT, 128), scalar1=self.comb[:, i, e:e + 1], scalar2=None, op0=ALU.mult),
                             ["cst", ("comb", i)], dgk)
                    self.mm(psi, self.PS[psi][:, ti * 128:(ti + 1) * 128], [(self.ones_bf, dg)], reads=["cstb"] + dgk)
                self.act(lambda a, o=cb[:, bb * 512:(bb + 1) * 512], p=self.PS[psi][:, :]: a.copy(out=o, in_=p), [("ps", psi)], self.kY(cbc, bb))
            self.ffn(self.d_m13[e], self.d_m2[e], cb=cb, cbk=lambda bb, cbc=cbc: self.kY(cbc, bb))

    def dense_ffn(self, l):
        self.rmsnorm(OP_GFFN + 8 * l, lambda kc, bb: self.H[:, kc, bb * 512:(bb + 1) * 512], lambda kc, bb: self.kH(kc, bb))
        self.ffn(self.d_f13, self.d_f2)

    def final(self):
        flat = self.ymflat()

        def out_fn(kc, bb):
            base = (bb % 2) * 8192
            return flat[:, base + kc * 1024:base + (kc + 1) * 1024].bitcast(F32)

        def out_keys(kc, bb):
            base = (bb % 2) * 8192
            return self.ymkeys(base + kc * 1024, 1024)

        def hook(bb, rs):
            for kc in range(8):
                self.b.dma("sp", "out%d_%d" % (bb % 2, kc), lambda q, kc=kc, bb=bb: q.dma_start(out=self.d_out[kc * 128:(kc + 1) * 128, bb * 512:(bb + 1) * 512], in_=out_fn(kc, bb)),
                           reads=out_keys(kc, bb), writes=[("out", kc, bb)])
        self.rmsnorm(OP_FIN, out_fn, out_keys, hook=hook)

    def finish(self):
        toks = []
        for k, t in self.b.lw.items():
            if isinstance(k, tuple) and k[0] in ("out", "dbg"):
                toks.append(t)
        self.b.wait_all("sp", toks)
        self.b.replay(self.nc, self.es)

    def program(self, skipP=False):
        self.init()
        if not skipP:
            self.load_x(self.d_xp)
            self.mixer(0, "pfull")
            self.dense_ffn(0)
            self.mixer(1, "pstate")
        else:
            for l in range(L):
                self.pool(lambda g_, l=l: g_.memset(self.pay[l][:], 0.0), [], [("pay", l)])
        self.load_x(self.d_x)
        for l in range(L):
            self.mixer(l, "main")
            if self.stop is not None and self.stop.endswith(str(l)) and self.stop[:-1] in ("norm", "branch", "gate", "mixer", "a1_", "a2_", "x_", "b_"):
                break
            if l % 2 == 0:
                self.dense_ffn(l)
            else:
                self.moe(l)
            self.dump("XF%d" % l, lambda kc: self.X[:, kc, :], lambda kc: [("X", kc, i) for i in range(NT)])
            if self.stop == "ffn%d" % l:
                break
        self.final()
        self.finish()


def _kmaj(w):
    K, N = w.shape
    return np.ascontiguousarray(w.reshape(K // 128, 128, N).transpose(1, 0, 2))


def _consts():
    p = np.arange(128)
    c = np.zeros((128, NC), np.float32)
    c[:, OC_IDENT:OC_IDENT + 128] = np.eye(128)
    s, t = p[:, None], p[None, :]
    c[:, OC_TRISGU:OC_TRISGU + 128] = (s <= t)
    same = (s // 64) == (t // 64)
    c[:, OC_CAUS2:OC_CAUS2 + 128] = same & (s <= t)
    hv = np.arange(256)[None, :]
    c[:, OC_BMASK:OC_BMASK + 256] = (p[:, None] // 32) == (hv // 64)
    c[:, OC_TRI2:OC_TRI2 + 128] = (same & (s <= t)) * (-1.0 / 16)
    c[:, OC_REV2:OC_REV2 + 128] = (same & (s > t)) * (-1.0 / 16)
    c[:, OC_CH2:OC_CH2 + 2] = ((p[:, None] // 64) == np.arange(2)[None, :]) * (-1.0 / 16)
    c[:, OC_ONES:OC_ONES + 128] = 1.0
    c[:, OC_HM:OC_HM + 4] = ((p[:, None] // 32) == np.arange(4)[None, :]) * (32 ** -0.5)
    return c


def _prep(inp, small=False):
    f = lambda a: np.ascontiguousarray(a, dtype=np.float32)
    w_in = inp["w_in"]
    out = {}
    out["cst"] = _consts()
    smP = np.zeros((128, NSP), np.float32)
    for l in range(L):
        smP[:, OP_GMIX + 8 * l:OP_GMIX + 8 * l + 8] = inp["norm_mix"][l].reshape(8, 128).T
        smP[:, OP_GFFN + 8 * l:OP_GFFN + 8 * l + 8] = inp["norm_ffn"][l].reshape(8, 128).T
        bg = inp["b_gate"][l].reshape(4, 8, 128)
        smP[:, OP_BGATE + 32 * l:OP_BGATE + 32 * l + 32] = bg.transpose(2, 0, 1).reshape(128, 32)
    smP[:, OP_FIN:OP_FIN + 8] = inp["final_norm"].reshape(8, 128).T
    smP[:, OP_ROUTER:OP_ROUTER + 64] = inp["moe_router"][0].reshape(8, 128, 8).transpose(1, 0, 2).reshape(128, 64)
    out["smP"] = smP
    smB = np.zeros((L, 128, NSB), np.float32)
    wins = (2, 4, 8, 16)
    for l in range(L):
        sb = inp["sgu_b"][l]
        smB[l, :, OB_SGUB:OB_SGUB + 256] = np.repeat(sb.reshape(2, 2, 128), 64, axis=1).transpose(1, 0, 2).reshape(128, 256)
        smB[l, :, OB_SGUN:OB_SGUN + 256] = inp["sgu_norm"][l][None, :]
        smB[l, :, OB_GLAN:OB_GLAN + 256] = np.tile(inp["gla_norm"][l], 4)[None, :]
        smB[l, :, OB_WST:OB_WST + 512] = inp["sgu_w"][l].transpose(2, 0, 1).reshape(128, 512)
        pw = np.zeros((128, 2, 128), np.float32)
        for g in range(4):
            cc, hh = g // 2, g % 2
            pw[64 * hh:64 * hh + 64, cc, 64 * hh:64 * hh + 64] = inp["pool_w"][l][g]
        smB[l, :, OB_POOLW:OB_POOLW + 256] = pw.reshape(128, 256)
        smB[l, :, OB_BG:OB_BG + 128] = inp["gla_bg"][l][None, :]
        smB[l, :, OB_CONVW:OB_CONVW + 6] = inp["conv_w"][l].reshape(3, 2, 128).transpose(2, 0, 1).reshape(128, 6)
        smB[l, :, OB_CONVB:OB_CONVB + 2] = inp["conv_b"][l].reshape(2, 128).T
        smB[l, :, OB_PSC:OB_PSC + 2] = inp["pool_scale"][l].reshape(2, 128).T
        for cc in range(2):
            for hh in range(2):
                smB[l, 64 * hh:64 * hh + 64, OB_INVW + cc] = 1.0 / wins[2 * cc + hh]
    out["smB"] = smB
    cols = lambda a, b_: list(range(a, b_))
    fm = cols(512, 768) + cols(1024, 1280) + cols(0, 256) + cols(768, 1024) + cols(1280, 1536)
    kv = cols(1664, 1792) + cols(1792, 2048)
    rv = cols(2048, 2304) + cols(256, 512)
    qk = cols(1536, 1664) + cols(1664, 1792)
    gc_ = []
    for j in range(8):
        for i in range(4):
            gc_ += cols(2320 + i * 1024 + j * 128, 2320 + i * 1024 + (j + 1) * 128)
    out["win_fm"] = np.stack([_kmaj(w_in[l][:, fm]) for l in range(L)])
    out["win_kv"] = np.stack([_kmaj(w_in[l][:, kv]) for l in range(L)])
    out["win_glrT"] = np.stack([f(w_in[l][:, 2304:2320].T) for l in range(L)])
    out["wg2"] = f(inp["gla_wg2"])
    out["win_rv"] = np.stack([_kmaj(w_in[l][:, rv]) for l in range(L)])
    out["win_qk"] = np.stack([_kmaj(w_in[l][:, qk]) for l in range(L)])
    out["win_g"] = np.stack([_kmaj(w_in[l][:, gc_]) for l in range(L)])
    out["bproj"] = np.stack([_kmaj(inp["branch_proj"][l].reshape(1024, 1024)) for l in range(L)])
    out["wout"] = np.stack([_kmaj(inp["w_out"][l]) for l in range(L)])

    def w13(w1, w3):
        a = np.empty((11, 128, 8, 512), np.float32)
        k1, k3 = _kmaj(w1), _kmaj(w3)
        for g in range(11):
            a[g, :, :, 0:256] = k1[:, :, 256 * g:256 * (g + 1)]
            a[g, :, :, 256:512] = k3[:, :, 256 * g:256 * (g + 1)]
        return a

    def w2t(w2):
        a = np.zeros((2, 4, 128, 12, 256), np.float32)
        k2 = _kmaj(w2)
        f0 = 0
        for fh, nf in enumerate((12, 10)):
            for dq in range(4):
                a[fh, dq, :, 0:nf, :] = k2[:, f0:f0 + nf, dq * 256:(dq + 1) * 256]
            f0 += nf
        return a

    out["ffn_w13"] = w13(inp["ffn_w1"][0], inp["ffn_w3"][0])
    out["ffn_w2t"] = w2t(inp["ffn_w2"][0])
    if small:
        out["moe_w13"] = np.zeros((1, 1, 1, 1, 8), np.float32)
        out["moe_w2t"] = np.zeros((1, 1, 1, 1, 1, 8), np.float32)
    else:
        out["moe_w13"] = np.stack([w13(inp["moe_w1"][0][e], inp["moe_w3"][0][e]) for e in range(NE)])
        out["moe_w2t"] = np.stack([w2t(inp["moe_w2"][0][e]) for e in range(NE)])
    return out


def _percore(x, c):
    bi, half = c // 2, c % 2
    xT = np.ascontiguousarray(x[bi, half * T:(half + 1) * T, :].T, dtype=np.float32)
    if half == 1:
        xTp = np.ascontiguousarray(x[bi, 0:T, :].T, dtype=np.float32)
    else:
        xTp = np.zeros((D, T), np.float32)
    pc = np.zeros((128, NPC), np.float32)
    pc[:, 0] = float(half)
    wins = (2, 4, 8, 16)
    t = np.arange(16)
    for cc in range(2):
        for hh in range(2):
            w = wins[2 * cc + hh]
            first = 1.0 / np.minimum(t + 1, w)
            v = first if half == 0 else np.full(16, 1.0 / w)
            pc[64 * hh:64 * hh + 64, 8 + 16 * cc:24 + 16 * cc] = v[None, :]
            pc[64 * hh:64 * hh + 64, 40 + 16 * cc:56 + 16 * cc] = first[None, :]
    return xT, xTp, pc


_NC_CACHE = {}


def build_nc(dbg=(), stop=None, skipP=False):
    key = (tuple(dbg), stop, skipP)
    if key in _NC_CACHE:
        return _NC_CACHE[key]
    nc = bass.Bass("TRN2", target_bir_lowering=False)
    es = ExitStack()
    mk = MK(nc, es, dbg=list(dbg), stop=stop)
    mk.program(skipP=skipP)
    es.close()
    _NC_CACHE[key] = nc
    return nc


def run(inputs, dbg=(), stop=None, trace=False, skipP=False):
    inp = {k: np.asarray(v) for k, v in inputs.items()}
    small = stop is not None and stop != "ffn1"
    shared = _prep(inp, small)
    in_maps = []
    for c in range(NCORES):
        xT, xTp, pc = _percore(inp["x"], c)
        m = dict(shared)
        m["xT"] = xT
        m["xTp"] = xTp
        m["pc"] = pc
        in_maps.append(m)
    nc = build_nc(dbg, stop, skipP)
    res = run_bass_kernel_spmd(nc, in_maps, core_ids=list(range(NCORES)), **({"trace": True} if trace else {}))
    return res


def kernel(**inputs):
    res = run(inputs)
    x = np.asarray(inputs["x"])
    out = np.empty(x.shape, np.float32)
    for c in range(NCORES):
        bi, half = c // 2, c % 2
        out[bi, half * T:(half + 1) * T, :] = res.results[c]["outT"].T
    return out
```
